# Optimizing a Trainium2 kernel written in Bass

```python
import jax, jax.numpy as jnp
from jax import lax
import numpy as np

D_MODEL = 1024
BATCH = 8
SEQ = 2048
DEPTH = 2

CHUNK = 64

D_A = D_MODEL
CONV_A_WIDTH = 31
D_B = D_MODEL
CONV_B_WIDTH = 3
D_C = D_MODEL
POOL_WINDOWS = (2, 4, 8, 16)
N_POOL_GROUPS = len(POOL_WINDOWS)
GROUP_C = D_C // N_POOL_GROUPS
N_BRANCHES = 3
IN_COLS = 2 * D_A + 3 * D_B + D_C + N_BRANCHES * D_MODEL

N_EXPERTS = 16
N_EXPERT_GROUPS = 4
EXPERTS_PER_GROUP = N_EXPERTS // N_EXPERT_GROUPS
TOP_K = 2
D_EXPERT = D_MODEL // 2

LN_EPS = 1e-5
DEEPNORM_ALPHA = (2 * DEPTH) ** 0.25
DEEPNORM_BETA = (8 * DEPTH) ** -0.25

kernel_name = "hybrid_conv_pool_gated_moe_trunk"


def layer_norm(x, g, b):
    x32 = x.astype(jnp.float32)
    mu = jnp.mean(x32, axis=-1, keepdims=True)
    xc = x32 - mu
    var = jnp.mean(xc * xc, axis=-1, keepdims=True)
    y = xc * lax.rsqrt(var + LN_EPS) * g.astype(jnp.float32) + b.astype(jnp.float32)
    return y.astype(x.dtype)


def causal_depthwise_conv(u, w):
    k, c = w.shape
    return lax.conv_general_dilated(
        u, w[:, None, :].astype(u.dtype), window_strides=(1,), padding=[(k - 1, 0)],
        dimension_numbers=("NWC", "WIO", "NWC"), feature_group_count=c)


def multi_scale_pool_minus_self(u):
    t_len = u.shape[1]
    u32 = u.astype(jnp.float32)
    cs = jnp.cumsum(u32, axis=1)
    t_idx = jnp.arange(t_len)
    outs = []
    for g, w in enumerate(POOL_WINDOWS):
        c = cs[..., g * GROUP_C:(g + 1) * GROUP_C]
        lower = jnp.pad(c, ((0, 0), (w, 0), (0, 0)))[:, :t_len]
        count = jnp.minimum(t_idx + 1, w).astype(jnp.float32)[None, :, None]
        outs.append((c - lower) / count - u32[..., g * GROUP_C:(g + 1) * GROUP_C])
    return jnp.concatenate(outs, axis=-1).astype(u.dtype)


def hybrid_mixer(x, w_in, b_in, conv_a_w, conv_a_b, ln_a_g, ln_a_b, w_a_out,
                 conv_b_w, w_b_out, w_c_group, c_scale, w_o, b_o):
    bsz, t_len, _ = x.shape
    proj = jnp.einsum("btd,dc->btc", x, w_in) + b_in
    o1 = 2 * D_A
    o2 = o1 + 3 * D_B
    o3 = o2 + D_C
    a_in, b_part, c_in, gate_in = jnp.split(proj, [o1, o2, o3], axis=-1)

    a_val, a_gate = jnp.split(a_in, 2, axis=-1)
    a = a_val * jax.nn.sigmoid(a_gate)
    a = causal_depthwise_conv(a, conv_a_w) + conv_a_b
    a = jax.nn.silu(layer_norm(a, ln_a_g, ln_a_b))
    y_a = jnp.einsum("btc,cd->btd", a, w_a_out)

    gate_bb, gate_cc, xb = jnp.split(b_part, 3, axis=-1)
    yb = gate_bb * causal_depthwise_conv(gate_cc * xb, conv_b_w)
    y_b = jnp.einsum("btc,cd->btd", yb, w_b_out)

    pooled = multi_scale_pool_minus_self(c_in).reshape(bsz, t_len, N_POOL_GROUPS, GROUP_C)
    y_c = jnp.einsum("btgi,gio->btgo", pooled, w_c_group).reshape(bsz, t_len, D_C) * c_scale

    g = jax.nn.sigmoid(gate_in).reshape(bsz, t_len, N_BRANCHES, D_MODEL)
    merged = g[..., 0, :] * y_a + g[..., 1, :] * y_b + g[..., 2, :] * y_c
    return jnp.einsum("btd,de->bte", merged, w_o) + b_o


def grouped_moe(x, w_router, b_router, w_exp_gate, w_exp_up, w_exp_down):
    bsz, t_len, d = x.shape
    xt = x.reshape(-1, d)
    logits = (xt @ w_router + b_router).astype(jnp.float32)
    probs = jax.nn.softmax(logits, axis=-1)
    grouped = probs.reshape(-1, N_EXPERT_GROUPS, EXPERTS_PER_GROUP)
    group_score = lax.top_k(grouped, TOP_K)[0].sum(-1)
    best_group = jnp.argmax(group_score, axis=-1)
    in_group = jnp.einsum("ng,nge->ne", jax.nn.one_hot(best_group, N_EXPERT_GROUPS, dtype=jnp.float32), grouped)
    top_p, top_i = lax.top_k(in_group, TOP_K)
    top_p = top_p / jnp.sum(top_p, axis=-1, keepdims=True)
    expert_idx = best_group[:, None] * EXPERTS_PER_GROUP + top_i
    combine = jnp.einsum("nk,nke->ne", top_p, jax.nn.one_hot(expert_idx, N_EXPERTS, dtype=jnp.float32)).astype(x.dtype)
    y = jnp.zeros_like(xt)
    for e in range(N_EXPERTS):
        h = jax.nn.silu(xt @ w_exp_gate[e]) * (xt @ w_exp_up[e])
        y = y + combine[:, e:e + 1] * (h @ w_exp_down[e])
    return y.reshape(bsz, t_len, d)


def setup_inputs(seed: int = 0) -> dict:
    key = jax.random.key(seed)
    ks = jax.random.split(key, 24)
    f32 = jnp.float32

    def nrm(k, shape, scale):
        return jax.random.normal(k, shape, f32) * scale

    L = DEPTH
    return {
        "x": nrm(ks[0], (BATCH, SEQ, D_MODEL), 1.0),
        "w_in": nrm(ks[1], (L, D_MODEL, IN_COLS), D_MODEL ** -0.5),
        "b_in": nrm(ks[2], (L, IN_COLS), 0.01),
        "conv_a_w": nrm(ks[3], (L, CONV_A_WIDTH, D_A), CONV_A_WIDTH ** -0.5),
        "conv_a_b": nrm(ks[4], (L, D_A), 0.01),
        "ln_a_g": 1.0 + nrm(ks[5], (L, D_A), 0.01),
        "ln_a_b": nrm(ks[6], (L, D_A), 0.01),
        "w_a_out": nrm(ks[7], (L, D_A, D_MODEL), D_A ** -0.5 * DEEPNORM_BETA),
        "conv_b_w": nrm(ks[8], (L, CONV_B_WIDTH, D_B), CONV_B_WIDTH ** -0.5),
        "w_b_out": nrm(ks[9], (L, D_B, D_MODEL), D_B ** -0.5 * DEEPNORM_BETA),
        "w_c_group": nrm(ks[10], (L, N_POOL_GROUPS, GROUP_C, GROUP_C), GROUP_C ** -0.5 * DEEPNORM_BETA),
        "c_scale": 1.0 + nrm(ks[11], (L, D_C), 0.01),
        "w_o": nrm(ks[12], (L, D_MODEL, D_MODEL), D_MODEL ** -0.5 * DEEPNORM_BETA),
        "b_o": nrm(ks[13], (L, D_MODEL), 0.01),
        "ln1_g": 1.0 + nrm(ks[14], (L, D_MODEL), 0.01),
        "ln1_b": nrm(ks[15], (L, D_MODEL), 0.01),
        "w_router": nrm(ks[16], (D_MODEL, N_EXPERTS), D_MODEL ** -0.5),
        "b_router": nrm(ks[17], (N_EXPERTS,), 0.01),
        "w_exp_gate": nrm(ks[18], (L, N_EXPERTS, D_MODEL, D_EXPERT), D_MODEL ** -0.5),
        "w_exp_up": nrm(ks[19], (L, N_EXPERTS, D_MODEL, D_EXPERT), D_MODEL ** -0.5),
        "w_exp_down": nrm(ks[20], (L, N_EXPERTS, D_EXPERT, D_MODEL), D_EXPERT ** -0.5 * DEEPNORM_BETA),
        "ln2_g": 1.0 + nrm(ks[21], (L, D_MODEL), 0.01),
        "ln2_b": nrm(ks[22], (L, D_MODEL), 0.01),
    }


def reference(x, w_in, b_in, conv_a_w, conv_a_b, ln_a_g, ln_a_b, w_a_out,
              conv_b_w, w_b_out, w_c_group, c_scale, w_o, b_o, ln1_g, ln1_b,
              w_router, b_router, w_exp_gate, w_exp_up, w_exp_down, ln2_g, ln2_b):
    for l in range(DEPTH):
        mix = hybrid_mixer(x, w_in[l], b_in[l], conv_a_w[l], conv_a_b[l], ln_a_g[l], ln_a_b[l],
                           w_a_out[l], conv_b_w[l], w_b_out[l], w_c_group[l], c_scale[l],
                           w_o[l], b_o[l])
        x = layer_norm(DEEPNORM_ALPHA * x + mix, ln1_g[l], ln1_b[l])
        ffn = grouped_moe(x, w_router, b_router, w_exp_gate[l], w_exp_up[l], w_exp_down[l])
        x = layer_norm(DEEPNORM_ALPHA * x + ffn, ln2_g[l], ln2_b[l])
    return x
```

```python
import numpy as np
from contextlib import ExitStack
import concourse.bass as bass
import concourse.mybir as mybir
from concourse.bass_utils import run_bass_kernel_spmd

F32 = mybir.dt.float32
BF16 = mybir.dt.bfloat16
AF = mybir.ActivationFunctionType
ALU = mybir.AluOpType
AX = mybir.AxisListType

D = 1024
T = 2048
TW = 512
NTT = T // TW
C = 8
DEPTH = 2
NE = 16
DE = 512
IN_COLS = 9216
ALPHA = float((2 * DEPTH) ** 0.25)
EPS = 1e-5


def _bf16_round(v):
    u = np.array([v], np.float32).view(np.uint32)
    u = (u + np.uint32(0x7FFF) + ((u >> np.uint32(16)) & np.uint32(1))) & np.uint32(0xFFFF0000)
    return float(u.view(np.float32)[0])


A_HI = _bf16_round(ALPHA)
A_LO = _bf16_round(ALPHA - A_HI)
NSLOT = 4
STRICT_SAME_ENGINE = True

P_BIN = 0
P_CAW = 72
P_CAB = 320
P_LAG = 328
P_LAB = 336
P_CBW = 344
P_CS = 368
P_BO = 376
P_L1G = 384
P_L1B = 392
P_L2G = 400
P_L2B = 408
NPL = 416


class Sched:
    def __init__(self, nc):
        self.nc = nc
        self.streams = {e: [] for e in ("pe", "act", "dve", "pool", "sp")}
        self.cnt = {}
        self.res = {}
        self.waited = {e: {} for e in self.streams}
        self.sems = {}

    def _deps(self, stream, reads, writes):
        deps = {}

        def add(sk, v, raw):
            if sk == stream and not raw and not STRICT_SAME_ENGINE:
                return
            if deps.get(sk, 0) < v:
                deps[sk] = v

        for r in reads:
            st = self.res.get(r)
            if st and st[0]:
                add(st[0][0], st[0][1], True)
        for w in writes:
            st = self.res.get(w)
            if st:
                if st[0]:
                    add(st[0][0], st[0][1], False)
                for sk, v in st[1].items():
                    add(sk, v, False)
        out = []
        wd = self.waited[stream]
        for sk, v in deps.items():
            if wd.get(sk, 0) >= v:
                continue
            wd[sk] = v
            out.append((sk, v))
        return out

    def _commit(self, sk, val, reads, writes):
        for r in reads:
            st = self.res.setdefault(r, [None, {}])
            st[1][sk] = val
        for w in writes:
            self.res[w] = [(sk, val), {}]

    def op(self, eng, fn, reads=(), writes=()):
        reads = list(reads)
        writes = list(writes)
        waits = self._deps(eng, reads, writes)
        val = self.cnt.get(eng, 0) + 1
        self.cnt[eng] = val
        self._commit(eng, val, reads, writes)
        self.streams[eng].append((waits, fn, eng, 1))
        return val

    def dma(self, stream, semkey, fn, reads=(), writes=()):
        reads = list(reads)
        writes = list(writes)
        waits = self._deps(stream, reads, writes)
        val = self.cnt.get(semkey, 0) + 16
        self.cnt[semkey] = val
        self._commit(semkey, val, reads, writes)
        self.streams[stream].append((waits, fn, semkey, 16))
        return val

    def barrier(self):
        allw = [(sk, v) for sk, v in self.cnt.items() if v > 0]
        for st in self.streams:
            if st == "pool":
                continue
            wd = self.waited[st]
            waits = []
            for sk, v in allw:
                if wd.get(sk, 0) < v:
                    wd[sk] = v
                    waits.append((sk, v))
            if waits:
                self.streams[st].append((waits, None, None, 0))
        self.res = {k: v for k, v in self.res.items() if isinstance(k, tuple) and k[0] == "w"}

    def final_wait(self, stream, semkeys):
        waits = [(sk, self.cnt[sk]) for sk in semkeys if self.cnt.get(sk, 0) > 0]
        self.streams[stream].append((waits, None, None, 0))

    def emit(self, E):
        nc = self.nc
        for sk in self.cnt:
            self.sems[sk] = E(nc.semaphore("s_" + str(sk)))
        block = E(nc.Block())
        sems = self.sems
        waited_vals = {}
        for st in self.streams.values():
            for waits, fn, sk, inc in st:
                for wsk, wv in waits:
                    waited_vals.setdefault(wsk, set()).add(wv)

        def run(stream):
            def body(eng):
                pend = 0
                cum = 0
                ops = self.streams[stream]
                last_idx = max([i for i, o in enumerate(ops) if o[1] is not None and o[2] == stream], default=-1)
                for idx, (waits, fn, sk, inc) in enumerate(ops):
                    for wsk, wv in waits:
                        eng.wait_ge(sems[wsk], wv)
                    if fn is None:
                        continue
                    ins = fn(eng)
                    if sk != stream:
                        ins.then_inc(sems[sk], inc)
                        continue
                    pend += inc
                    cum += inc
                    if cum in waited_vals.get(sk, ()) or idx == last_idx:
                        ins.then_inc(sems[sk], pend)
                        pend = 0
            return body

        block.sync(run("sp"))
        block.tensor(run("pe"))
        block.scalar(run("act"))
        block.vector(run("dve"))
        block.gpsimd(run("pool"))


def build_program(n_layers=DEPTH, debug=False):
    nc = bass.Bass("TRN2", target_bir_lowering=False)
    dram = lambda name, shape, kind="ExternalInput", dt=F32: nc.dram_tensor(name, shape, dt, kind=kind).ap()
    xT_d = dram("xT", [D, T])
    w_in_d = dram("w_in", [DEPTH, D, IN_COLS])
    w_a_d = dram("w_a_out", [DEPTH, D, D])
    w_b_d = dram("w_b_out", [DEPTH, D, D])
    w_c_d = dram("w_c_group", [DEPTH, 4, 256, 256])
    w_o_d = dram("w_o", [DEPTH, D, D])
    w_g_d = dram("w_exp_gate", [DEPTH, NE, D, DE])
    w_u_d = dram("w_exp_up", [DEPTH, NE, D, DE])
    w_d_d = dram("w_exp_down", [DEPTH, NE, DE, D])
    par_d = dram("params", [128, DEPTH * NPL])
    wr_d = dram("wr", [128, C * NE])
    br_d = dram("br", [128, NE])
    y_d = dram("yT", [128, C * T], kind="ExternalOutput")
    dbg_d = {}
    if debug:
        dbg_d["d_xh"] = dram("d_xh", [128, C * T], kind="ExternalOutput", dt=BF16)
        dbg_d["d_xl"] = dram("d_xl", [128, C * T], kind="ExternalOutput", dt=BF16)
        dbg_d["d_comb"] = dram("d_comb", [128, 256], kind="ExternalOutput")

    S = Sched(nc)
    with ExitStack() as es:
        E = es.enter_context
        sb = lambda name, shape, dt: E(nc.sbuf_tensor(name, shape, dt))
        XH_t = sb("XH", [128, C, T], BF16)
        XL_t = sb("XL", [128, C, T], BF16)
        R64_t = sb("R64", [128, C * T], F32)
        WS_t = [sb("WS%d" % i, [128, 8 * 512], BF16) for i in range(NSLOT)]
        PAR_t = sb("PAR", [128, DEPTH * NPL], F32)
        WR_t = sb("WRF", [128, C * NE], F32)
        WRH_t = sb("WRH", [128, C * NE], BF16)
        WRL_t = sb("WRL", [128, C * NE], BF16)
        BR_t = sb("BR", [128, NE], F32)
        IDENT_t = sb("IDENT", [128, 128], F32)
        ONESB_t = sb("ONESB", [128, 128], BF16)
        IDENTB_t = sb("IDENTB", [128, 128], BF16)
        ALH_t = sb("ALH", [128, 128], BF16)
        ALL_t = sb("ALL", [128, 128], BF16)
        ONESF_t = sb("ONESF", [128, 128], F32)
        EPS_t = sb("EPSC", [128, 1], F32)
        INVC_t = sb("INVC", [128, 4 * 16], F32)
        SCRW = 10368
        SCR_t = sb("SCR", [128, SCRW], F32)
        PS = [E(nc.psum_tensor("ps%d" % i, [128, TW], F32)) for i in range(8)]

        XH = XH_t[:]
        XL = XL_t[:]
        ACC = R64_t[:].rearrange("p (c t) -> p c t", c=C)
        r64b = R64_t[:].bitcast(BF16).rearrange("p (h c t) -> p h c t", h=2, c=C)
        BIG1 = r64b[:, 0]
        MERGED = r64b[:, 1]
        PAR = PAR_t[:]
        INVC = INVC_t[:].rearrange("p (g t) -> p g t", g=4)
        WRH = WRH_t[:].rearrange("p (k e) -> p k e", k=C)
        WRL = WRL_t[:].rearrange("p (k e) -> p k e", k=C)

        PAD = 32
        TLEN = PAD + T

        def scr(off, n, dt=F32):
            if dt == F32:
                return SCR_t[:, off:off + n]
            return SCR_t[:, off:off + (n + 1) // 2].bitcast(BF16)

        o = 0
        TA = scr(o, TLEN); o += TLEN
        TB = scr(o, TLEN); APAD2 = [scr(o, TLEN, BF16), scr(o + TLEN // 2, TLEN, BF16)]; o += TLEN
        TC = scr(o, TLEN); o += TLEN
        GT2 = [scr(o + i * 256, TW, BF16) for i in range(2)]; o += 512
        SQ2 = [scr(o + i * 256, TW, BF16) for i in range(2)]; o += 512
        SM = [scr(o + i * TW, TW) for i in range(6)]; o += 6 * TW
        assert o <= SCRW, o
        KD = 11
        NKP = 31 - KD
        DG = [scr(3 * TLEN + 1024, NKP * 128, BF16), scr(3 * TLEN + 1024 + 1536, NKP * 128, BF16)]
        DGn = [["sm0", "sm1", "sm2"], ["sm3", "sm4", "sm5"]]
        o = 0
        RT = [scr(o + i * 256, 256) for i in range(12)]; o += 12 * 256
        CBb = [scr(o, TW, BF16), scr(o + TW // 2, TW, BF16)]; o += TW
        Hb = [scr(o + i * 1024, 4 * TW, BF16).rearrange("p (c t) -> p c t", c=4) for i in range(2)]; o += 2048
        MS = [scr(o + i * TW, TW) for i in range(6)]; o += 6 * TW
        assert o <= SCRW, o

        bank_ctr = [0]
        nrot = [6]

        def nbank():
            b = bank_ctr[0] % nrot[0]
            bank_ctr[0] += 1
            return b

        slot_ctr = [0]
        live = {}
        slot_key = [None] * NSLOT

        def wget(key, src_ap, shape3):
            if key in live:
                s = live[key]
            else:
                s = slot_ctr[0] % NSLOT
                slot_ctr[0] += 1
                if slot_key[s] is not None:
                    del live[slot_key[s]]
                slot_key[s] = key
                live[key] = s
                k, n = shape3
                view = WS_t[s][:, 0:k * n].rearrange("p (k n) -> p k n", k=k)
                S.dma("pool", "w%d" % s, lambda e, view=view, src=src_ap: e.dma_start(out=view, in_=src),
                      writes=[("w", s)])
            k, n = shape3
            return s, WS_t[s][:, 0:k * n].rearrange("p (k n) -> p k n", k=k)

        def win_piece(l, p):
            src = w_in_d[l, :, p * 512:(p + 1) * 512].rearrange("(k p) n -> p k n", p=128)
            return wget(("win", l, p), src, (8, 512))

        def sq_piece(name, dram_ap, l, p):
            src = dram_ap[l, :, p * 512:(p + 1) * 512].rearrange("(k p) n -> p k n", p=128)
            return wget((name, l, p), src, (8, 512))

        def mm_group(out_ap, pairs, reads, bank):
            def fn(e):
                n = len(pairs)
                ins = None
                for i, (lh, rh) in enumerate(pairs):
                    ins = e.matmul(out_ap, lh, rh, start=(i == 0), stop=(i == n - 1))
                return ins
            S.op("pe", fn, reads=reads, writes=[("ps", bank)])

        def tsl(tt):
            return slice(tt * TW, (tt + 1) * TW)

        def proj(l, j, tt):
            s, wv = win_piece(l, j // 4)
            jj = j % 4
            b = nbank()
            mm_group(PS[b][:], [(wv[:, k, jj * 128:(jj + 1) * 128], XH[:, k, tsl(tt)]) for k in range(C)],
                     [("w", s)] + [("XH", k, tt) for k in range(C)], b)
            return b

        def pcol(l, off):
            return PAR[:, l * NPL + off:l * NPL + off + 1]

        def act(out, in_, func, reads, writes, bias=None, scale=1.0):
            kw = {}
            if bias is not None:
                kw["bias"] = bias
            S.op("act", lambda e: e.activation(out=out, in_=in_, func=func, scale=scale, **kw), reads=reads, writes=writes)

        def tt_op(eng, out, in0, in1, op, reads, writes):
            S.op(eng, lambda e: e.tensor_tensor(out=out, in0=in0, in1=in1, op=op), reads=reads, writes=writes)

        def stt(eng, out, in0, scalar, in1, op0, op1, reads, writes):
            S.op(eng, lambda e: e.scalar_tensor_tensor(out=out, in0=in0, scalar=scalar, in1=in1, op0=op0, op1=op1),
                 reads=reads, writes=writes)

        def ts_op(eng, out, in0, s1, s2, op0, op1, reads, writes):
            if s2 is None:
                S.op(eng, lambda e: e.tensor_scalar(out=out, in0=in0, scalar1=s1, scalar2=None, op0=op0), reads=reads, writes=writes)
            else:
                S.op(eng, lambda e: e.tensor_scalar(out=out, in0=in0, scalar1=s1, scalar2=s2, op0=op0, op1=op1),
                     reads=reads, writes=writes)

        BM, BQ = 6, 7

        def ln_mean(pairs, reads, bm=BM):
            mm_group(PS[bm][:], pairs, reads, bm)

        def ln_sq(src, reads, i, n, ones, sq, bq=BQ, sqn="sqt"):
            q = sq[i % 2]
            qn = sqn + str(i % 2)
            act(q, src, AF.Square, reads, [qn])
            S.op("pe", lambda e: e.matmul(PS[bq][:], ones, q, start=(i == 0), stop=(i == n - 1)),
                 reads=["consts", qn], writes=[("ps", bq)])

        def ln_finish(sm_mean, sm_rstd, sm_mr, sm_tmp, names, bm=BM, bq=BQ):
            nm, nr, nmr, nt = names[0], names[1], names[2], names[3]
            act(sm_mean, PS[bm][:], AF.Copy, [("ps", bm)], [nm])
            tt_op("dve", sm_tmp, sm_mean, sm_mean, ALU.mult, [nm], [nt])
            tt_op("dve", sm_tmp, PS[bq][:], sm_tmp, ALU.subtract, [("ps", bq), nt], [nt])
            ts_op("dve", sm_tmp, sm_tmp, 0.0, None, ALU.max, None, [nt], [nt])
            act(sm_rstd, sm_tmp, AF.Sqrt, [nt, "consts"], [nr], bias=EPS_t[:, 0:1])
            S.op("dve", lambda e: e.reciprocal(out=sm_rstd, in_=sm_rstd), reads=[nr], writes=[nr])
            tt_op("dve", sm_mr, sm_mean, sm_rstd, ALU.mult, [nm, nr], [nmr])

        HILO_ENG = ["dve"]

        def split_hilo(src_f32, c, tt, src_res):
            S.op("act", lambda e: e.copy(out=XH[:, c, tsl(tt)], in_=src_f32), reads=[src_res], writes=[("XH", c, tt)])
            tt_op(HILO_ENG[0], XL[:, c, tsl(tt)], src_f32, XH[:, c, tsl(tt)], ALU.subtract, [src_res, ("XH", c, tt)], [("XL", c, tt)])

        S.dma("sp", "ld0", lambda e: e.dma_start(out=PAR, in_=par_d), writes=["par"])
        S.dma("sp", "ld1", lambda e: e.dma_start(out=WR_t[:], in_=wr_d), writes=["wrf"])
        S.dma("sp", "ld2", lambda e: e.dma_start(out=BR_t[:], in_=br_d), writes=["br"])

        S.op("pool", lambda e: e.memset(IDENT_t[:], 0.0), writes=["ident0"])
        S.op("pool", lambda e: e.affine_select(out=IDENT_t[:], in_=IDENT_t[:], compare_op=ALU.not_equal, fill=1.0, base=0,
                                               pattern=[[-1, 128]], channel_multiplier=1), reads=["ident0"], writes=["ident0"])

        def consts(e):
            e.memset(ONESB_t[:], 1.0 / D)
            e.memset(ONESF_t[:], 1.0 / D)
            e.memset(EPS_t[:], EPS)
            for g, w in enumerate((2, 4, 8, 16)):
                e.memset(INVC[:, g, w - 1:16], 1.0 / w)
                for t in range(w - 1):
                    e.memset(INVC[:, g, t:t + 1], 1.0 / (t + 1))
            return e.memset(SCR_t[:], 0.0)
        S.op("pool", consts, reads=["ident0"], writes=["consts", "scrzero"])
        S.op("dve", lambda e: e.tensor_copy(out=IDENTB_t[:], in_=IDENT_t[:]), reads=["consts"], writes=["identb"])
        S.op("dve", lambda e: e.tensor_scalar(out=ALH_t[:], in0=IDENTB_t[:], scalar1=A_HI, scalar2=None, op0=ALU.mult),
             reads=["identb"], writes=["alh"])
        S.op("dve", lambda e: e.tensor_scalar(out=ALL_t[:], in0=IDENTB_t[:], scalar1=A_LO, scalar2=None, op0=ALU.mult),
             reads=["identb"], writes=["all"])
        S.op("act", lambda e: e.copy(out=WRH_t[:], in_=WR_t[:]), reads=["wrf"], writes=["wrh"])
        tt_op("dve", WRL_t[:], WR_t[:], WRH_t[:], ALU.subtract, ["wrf", "wrh"], ["wrl"])

        XST = R64_t[:].rearrange("p (b k t) -> p b k t", b=NTT, k=C)
        xsrc = xT_d.rearrange("(k p) t -> p k t", p=128)
        for tt in range(NTT):
            rk = "xin%d" % tt
            S.dma("sp", "ldx%d" % tt, lambda e, tt=tt: e.dma_start(out=XST[:, tt], in_=xsrc[:, :, tsl(tt)]), writes=[rk])
            S.op("act", lambda e, tt=tt: e.copy(out=XH[:, :, tsl(tt)], in_=XST[:, tt]), reads=[rk],
                 writes=[("XH", c, tt) for c in range(C)] + ["xconv"])
            tt_op("dve", XL[:, :, tsl(tt)], XST[:, tt], XH[:, :, tsl(tt)], ALU.subtract, [rk] + [("XH", c, tt) for c in range(C)],
                  [("XL", c, tt) for c in range(C)] + ["xconv"])

        SMn = ["sm%d" % i for i in range(6)]
        MSn = ["ms%d" % i for i in range(6)]
        git = [0]

        def gate(l, j, tt):
            i = git[0] % 2
            git[0] += 1
            bg = proj(l, j, tt)
            act(GT2[i], PS[bg][:], AF.Sigmoid, [("ps", bg), "par"], ["gt%d" % i], bias=pcol(l, P_BIN + j))
            return GT2[i], "gt%d" % i

        def pslice(buf, tt):
            return buf[:, PAD + tt * TW:PAD + (tt + 1) * TW]

        for l in range(n_layers):
            if l > 0:
                S.barrier()
                S.op("dve", lambda e: e.memset(SCR_t[:, 0:3 * TLEN], 0.0), writes=["TA", "TB", "TC"] + ["ap%d_%d" % (i, t) for i in range(2) for t in range(NTT)] + ["ta%d" % t for t in range(NTT)])

            nrot[0] = 4
            for c in range(C):
                dg, dgn = DG[c % 2], DGn[c % 2]
                APAD = APAD2[c % 2]
                apn = ["ap%d_%d" % (c % 2, t) for t in range(NTT)]

                def mkdiag(e, c=c, dg=dg, l=l):
                    ins = None
                    for k in range(KD, 31):
                        ins = e.activation(out=dg[:, (k - KD) * 128:(k - KD + 1) * 128], in_=IDENTB_t[:], func=AF.Copy,
                                           scale=pcol(l, P_CAW + k * 8 + c))
                    return ins
                S.op("act", mkdiag, reads=["identb", "par"], writes=dgn)
                x0 = ["scrzero"] if l == 0 else []
                for tt in range(NTT):
                    b = proj(l, c, tt)
                    act(pslice(TA, tt), PS[b][:], AF.Identity, [("ps", b), "par"] + x0, ["ta%d" % tt], bias=pcol(l, P_BIN + c))
                    b = proj(l, 8 + c, tt)
                    act(pslice(APAD, tt), PS[b][:], AF.Sigmoid, [("ps", b), "par"] + x0, [apn[tt]], bias=pcol(l, P_BIN + 8 + c))
                    tt_op("dve", pslice(APAD, tt), pslice(TA, tt), pslice(APAD, tt), ALU.mult, ["ta%d" % tt, apn[tt]], [apn[tt]])
                cb_ = []
                for tt in range(NTT):
                    b = 4 + tt
                    cb_.append(b)
                    mm_group(PS[b][:], [(dg[:, (k - KD) * 128:(k - KD + 1) * 128],
                                         APAD[:, PAD - 30 + k + tt * TW:PAD - 30 + k + (tt + 1) * TW]) for k in range(KD, 31)],
                             [apn[tt]] + ([apn[tt - 1]] if tt > 0 else []) + dgn, b)
                apall = list(apn)
                ts_op("dve", TC[:, PAD:], APAD[:, PAD - 30:PAD - 30 + T], pcol(l, P_CAW + 0 * 8 + c), pcol(l, P_CAB + c),
                      ALU.mult, ALU.add, apall + ["par"] + x0, ["TC"])
                for k in range(1, KD):
                    stt("dve", TC[:, PAD:], APAD[:, PAD - 30 + k:PAD - 30 + k + T], pcol(l, P_CAW + k * 8 + c), TC[:, PAD:],
                        ALU.mult, ALU.add, apall + ["TC", "par"], ["TC"])
                for tt in range(NTT):
                    b = cb_[tt]
                    tt_op("dve", BIG1[:, c, tsl(tt)], PS[b][:], pslice(TC, tt), ALU.add,
                          [("ps", b), "TC"] + (["xconv"] if l == 0 else []), [("BIG1", c, tt)])
            nrot[0] = 6
            wa = [sq_piece("wa", w_a_d, l, p) for p in range(2)]

            RS = [(pslice(TA, 0), "ta0"), (pslice(TA, 1), "ta1")]
            MR = [(pslice(TA, 2), "ta2"), (pslice(TA, 3), "ta3")]

            def st_mean(tt):
                ln_mean([(ONESB_t[:], BIG1[:, c, tsl(tt)]) for c in range(C)], ["consts"] + [("BIG1", c, tt) for c in range(C)])

            def st_sq(tt, c):
                q, qn = SQ2[c % 2], "sqt%d" % (c % 2)
                act(q, BIG1[:, c, tsl(tt)], AF.Square, [("BIG1", c, tt)], [qn])

            def st_sqmm(tt, c):
                q, qn = SQ2[c % 2], "sqt%d" % (c % 2)
                S.op("pe", lambda e: e.matmul(PS[BQ][:], ONESB_t[:], q, start=(c == 0), stop=(c == C - 1)),
                     reads=["consts", qn], writes=[("ps", BQ)])

            def st_fin(tt):
                (r, rn), (m, mn) = RS[tt % 2], MR[tt % 2]
                ln_finish(SM[0], r, m, SM[3], ["sm0", rn, mn, "sm3"])

            def lna_stats(tt):
                st_mean(tt)
                for c in range(C):
                    st_sq(tt, c)
                    st_sqmm(tt, c)
                st_fin(tt)

            NA = [(SM[4], "sm4"), (SM[5], "sm5")]
            abank = {}

            def nn0(tt, c):
                a, an = NA[c % 2]
                (r, rn), (m, mn) = RS[tt % 2], MR[tt % 2]
                tt_op("dve", a, BIG1[:, c, tsl(tt)], r, ALU.mult, [("BIG1", c, tt), rn], [an])
                tt_op("dve", a, a, m, ALU.subtract, [an, mn], [an])

            def nn1(tt, c, l=l):
                a, an = NA[c % 2]
                act(BIG1[:, c, tsl(tt)], a, AF.Silu, [an, "par"], [("BIG1", c, tt)],
                    bias=pcol(l, P_LAB + c), scale=pcol(l, P_LAG + c))

            def aa0(tt, oc, l=l):
                s_, wv = wa[oc // 4]
                bg = proj(l, 48 + oc, tt)
                b = nbank()
                oo = oc % 4
                mm_group(PS[b][:], [(wv[:, k, oo * 128:(oo + 1) * 128], BIG1[:, k, tsl(tt)]) for k in range(C)],
                         [("w", s_)] + [("BIG1", k, tt) for k in range(C)], b)
                abank[(tt, oc)] = (bg, b)

            def aa1(tt, oc, l=l):
                bg, b = abank[(tt, oc)]
                i = oc % 2
                act(GT2[i], PS[bg][:], AF.Sigmoid, [("ps", bg), "par"], ["gt%d" % i], bias=pcol(l, P_BIN + 48 + oc))

            def aa2(tt, oc):
                bg, b = abank[(tt, oc)]
                i = oc % 2
                tt_op("dve", MERGED[:, oc, tsl(tt)], PS[b][:], GT2[i], ALU.mult, [("ps", b), "gt%d" % i], [("MG", oc, tt)])

            lna_stats(0)
            for step in range(C + 1):
                if 0 <= step - 1 < C:
                    nn1(0, step - 1)
                if step < C:
                    nn0(0, step)
            lna_stats(1)
            for tt in range(NTT):
                nxt = tt + 1 < NTT
                nx2 = tt + 2 < NTT
                for step in range(C + 2):
                    if 0 <= step - 2 < C:
                        aa2(tt, step - 2)
                    if 0 <= step - 1 < C:
                        aa1(tt, step - 1)
                        if nxt:
                            nn1(tt + 1, step - 1)
                        if nx2:
                            st_sqmm(tt + 2, step - 1)
                    if step < C:
                        if nxt:
                            nn0(tt + 1, step)
                        if nx2:
                            if step == 0:
                                st_mean(tt + 2)
                            st_sq(tt + 2, step)
                        aa0(tt, step)
                if nx2:
                    st_fin(tt + 2)

            S.op("dve", lambda e: e.memset(SM[5][:, 0:1], 0.0), writes=["TA", "TB", "sm5"] + ["ap%d_%d" % (i, t) for i in range(2) for t in range(NTT)] + ["ta%d" % t for t in range(NTT)])
            for c in range(C):
                for tt in range(NTT):
                    b = proj(l, 24 + c, tt)
                    act(pslice(TA, tt), PS[b][:], AF.Identity, [("ps", b), "par"], ["TA"], bias=pcol(l, P_BIN + 24 + c))
                    b = proj(l, 32 + c, tt)
                    stt("dve", pslice(TB, tt), PS[b][:], pcol(l, P_BIN + 32 + c), pslice(TA, tt), ALU.add, ALU.mult,
                        [("ps", b), "TA", "par"], ["TB"])
                ts_op("dve", TC[:, PAD:], TB[:, PAD - 2:PAD - 2 + T], pcol(l, P_CBW + 0 * 8 + c), None, ALU.mult, None,
                      ["TB", "par"], ["TC"])
                for k in (1, 2):
                    stt("dve", TC[:, PAD:], TB[:, PAD - 2 + k:PAD - 2 + k + T], pcol(l, P_CBW + k * 8 + c), TC[:, PAD:],
                        ALU.mult, ALU.add, ["TB", "TC", "par"], ["TC"])
                for tt in range(NTT):
                    b = proj(l, 16 + c, tt)
                    stt("dve", BIG1[:, c, tsl(tt)], PS[b][:], pcol(l, P_BIN + 16 + c), pslice(TC, tt),
                        ALU.add, ALU.mult, [("ps", b), "TC", "par"], [("BIG1", c, tt)])
            for oc in range(C):
                s, wv = sq_piece("wb", w_b_d, l, oc // 4)
                for tt in range(NTT):
                    gt, gtn = gate(l, 56 + oc, tt)
                    b = nbank()
                    oo = oc % 4
                    mm_group(PS[b][:], [(wv[:, k, oo * 128:(oo + 1) * 128], BIG1[:, k, tsl(tt)]) for k in range(C)],
                             [("w", s)] + [("BIG1", k, tt) for k in range(C)], b)
                    tt_op("dve", SM[3], PS[b][:], gt, ALU.mult, [("ps", b), gtn], ["sm3"])
                    tt_op("dve", MERGED[:, oc, tsl(tt)], MERGED[:, oc, tsl(tt)], SM[3], ALU.add, [("MG", oc, tt), "sm3"], [("MG", oc, tt)])

            for g, w in enumerate((2, 4, 8, 16)):
                for c in (2 * g, 2 * g + 1):
                    for tt in range(NTT):
                        b = proj(l, 40 + c, tt)
                        act(pslice(TA, tt), PS[b][:], AF.Identity, [("ps", b), "par"], ["TA"], bias=pcol(l, P_BIN + 40 + c))
                    src, srcn = TA, "TA"
                    bufs = [(TB, "TB"), (TC, "TC")]
                    sh = 1
                    i = 0
                    while sh < w:
                        dst, dstn = bufs[i % 2]
                        tt_op("dve", dst[:, PAD:], src[:, PAD:], src[:, PAD - sh:PAD - sh + T], ALU.add, [srcn], [dstn])
                        src, srcn = dst, dstn
                        sh *= 2
                        i += 1
                    stt("dve", BIG1[:, c, :], src[:, PAD:], 1.0 / w, TA[:, PAD:], ALU.mult, ALU.subtract, [srcn, "TA"], [("BIG1", c, t4) for t4 in range(NTT)])
                    tt_op("dve", SM[3][:, 0:16], src[:, PAD:PAD + 16], INVC[:, g, :], ALU.mult, [srcn, "consts"], ["sm3"])
                    tt_op("dve", BIG1[:, c, 0:16], SM[3][:, 0:16], TA[:, PAD:PAD + 16], ALU.subtract, ["sm3", "TA", ("BIG1", c, 0)], [("BIG1", c, 0)])
            srcw = w_c_d[l].rearrange("g (k p) n -> p (g k) n", p=128)
            s, wv = wget(("wc", l), srcw, (8, 256))
            for oc in range(C):
                g = oc // 2
                for tt in range(NTT):
                    gt, gtn = gate(l, 64 + oc, tt)
                    b = nbank()
                    oo = oc % 2
                    mm_group(PS[b][:], [(wv[:, 2 * g + k, oo * 128:(oo + 1) * 128], BIG1[:, 2 * g + k, tsl(tt)]) for k in range(2)],
                             [("w", s)] + [("BIG1", 2 * g + k, tt) for k in range(2)], b)
                    stt("dve", SM[3], PS[b][:], pcol(l, P_CS + oc), gt, ALU.mult, ALU.mult, [("ps", b), gtn, "par"], ["sm3"])
                    tt_op("dve", MERGED[:, oc, tsl(tt)], MERGED[:, oc, tsl(tt)], SM[3], ALU.add, [("MG", oc, tt), "sm3"], [("MG", oc, tt)])

            so0, wo0 = sq_piece("wo", w_o_d, l, 0)
            so1, wo1 = sq_piece("wo", w_o_d, l, 1)
            nrot[0] = 4
            HILO_ENG[0] = "pool"

            VT = [(TA[:, i * TW:(i + 1) * TW], "va%d" % i) for i in range(4)] + [(TC[:, i * TW:(i + 1) * TW], "vc%d" % i) for i in range(4)]
            NT = [(TB[:, i * TW:(i + 1) * TW], "na%d" % i) for i in range(4)] + [(SM[4], "sm4"), (SM[5], "sm5")]
            SQ4 = [(SQ2[0], "sqt0"), (SQ2[1], "sqt1"), (GT2[0], "gt0"), (GT2[1], "gt1")]
            NI = NTT * C
            obank = {}

            def stat_banks(tt):
                return (4, 5) if tt % 2 == 0 else (6, 7)

            def c0(i, l=l):
                tt, oc = divmod(i, C)
                s, wv = (so0, wo0) if oc < 4 else (so1, wo1)
                oo = oc % 4
                b = nbank()
                obank[i] = b
                mm_group(PS[b][:], [(wv[:, k, oo * 128:(oo + 1) * 128], MERGED[:, k, tsl(tt)]) for k in range(C)]
                         + [(ALH_t[:], XH[:, oc, tsl(tt)]), (ALH_t[:], XL[:, oc, tsl(tt)]), (ALL_t[:], XH[:, oc, tsl(tt)])],
                         [("w", s), "alh", "all", ("XH", oc, tt), ("XL", oc, tt)] + [("MG", k, tt) for k in range(C)], b)

            def c1(i, l=l):
                tt, oc = divmod(i, C)
                v, vn = VT[i % 8]
                extra = (["TA"] if i < 4 else ["TC"]) if i < 8 else []
                b = obank[i]
                act(v, PS[b][:], AF.Identity, [("ps", b), "par"], [vn] + extra, bias=pcol(l, P_BO + oc))

            def c2(i):
                pass

            def c3(i):
                tt, oc = divmod(i, C)
                v, vn = VT[i % 8]
                q, qn = SQ4[i % 4]
                act(q, v, AF.Square, [vn], [qn])
                S.op("act", lambda e: e.copy(out=XH[:, oc, tsl(tt)], in_=v), reads=[vn], writes=[("XH", oc, tt)])

            def c4(i):
                tt, oc = divmod(i, C)
                v, vn = VT[i % 8]
                q, qn = SQ4[i % 4]
                bm, bq = stat_banks(tt)
                tt_op("pool", XL[:, oc, tsl(tt)], v, XH[:, oc, tsl(tt)], ALU.subtract, [vn, ("XH", oc, tt)], [("XL", oc, tt)])
                S.op("pe", lambda e: e.matmul(PS[bq][:], ONESB_t[:], q, start=(oc == 0), stop=(oc == C - 1)),
                     reads=["consts", qn], writes=[("ps", bq)])

            def c5(i):
                tt, oc = divmod(i, C)
                if oc == C - 1:
                    bm, bq = stat_banks(tt)
                    ln_mean([(ONESB_t[:], XH[:, c, tsl(tt)]) for c in range(C)] + [(ONESB_t[:], XL[:, c, tsl(tt)]) for c in range(C)],
                            ["consts"] + [("XH", c, tt) for c in range(C)] + [("XL", c, tt) for c in range(C)], bm)

            def n0(j):
                tt, c = divmod(j, C)
                if c == 0:
                    bm, bq = stat_banks(tt)
                    ln_finish(SM[0], SM[1], SM[2], SM[3], SMn, bm, bq)
                a, an = NT[j % 6]
                extra = ["TB"] if j < 4 else []
                tt_op("pool", a, XH[:, c, tsl(tt)], XL[:, c, tsl(tt)], ALU.add, [("XH", c, tt), ("XL", c, tt)], [an] + extra)

            def n1(j):
                a, an = NT[j % 6]
                tt_op("dve", a, a, SM[1], ALU.mult, [an, "sm1"], [an])
                tt_op("dve", a, a, SM[2], ALU.subtract, [an, "sm2"], [an])

            def n2(j, l=l):
                tt, c = divmod(j, C)
                a, an = NT[j % 6]
                act(a, a, AF.Identity, [an, "par"], [an], bias=pcol(l, P_L1B + c), scale=pcol(l, P_L1G + c))
                S.op("act", lambda e: e.copy(out=XH[:, c, tsl(tt)], in_=a), reads=[an], writes=[("XH", c, tt)])

            def n3(j):
                tt, c = divmod(j, C)
                a, an = NT[j % 6]
                tt_op("pool", XL[:, c, tsl(tt)], a, XH[:, c, tsl(tt)], ALU.subtract, [an, ("XH", c, tt)], [("XL", c, tt)])

            cst = [c0, c1, c2, c3, c4, c5]
            nst = [n0, n1, n2, n3]
            OFF = C + len(cst)
            for step in range(NI + OFF + len(nst)):
                for k in reversed(range(len(nst))):
                    j = step - OFF - k
                    if 0 <= j < NI:
                        nst[k](j)
                for k in reversed(range(len(cst))):
                    i = step - k
                    if 0 <= i < NI:
                        cst[k](i)
            nrot[0] = 6
            HILO_ENG[0] = "dve"

            if debug and l == 0:
                S.dma("sp", "st", lambda e: e.dma_start(out=dbg_d["d_xh"], in_=XH_t[:].rearrange("p c t -> p (c t)")),
                      reads=[("XH", c, tt) for c in range(C) for tt in range(NTT)])
                S.dma("sp", "st", lambda e: e.dma_start(out=dbg_d["d_xl"], in_=XL_t[:].rearrange("p c t -> p (c t)")),
                      reads=[("XL", c, tt) for c in range(C) for tt in range(NTT)])

            S.barrier()
            bl = nbank()
            NJ = T // 128

            def router(e, bl=bl):
                ins = None
                for j in range(NJ):
                    trip = []
                    for k in range(C):
                        trip.append((XH[:, k, j * 128:(j + 1) * 128], WRH[:, k, :]))
                        trip.append((XL[:, k, j * 128:(j + 1) * 128], WRH[:, k, :]))
                        trip.append((XH[:, k, j * 128:(j + 1) * 128], WRL[:, k, :]))
                    for i, (lh, rh) in enumerate(trip):
                        ins = e.matmul(PS[bl][:, j * NE:(j + 1) * NE], lh, rh, start=(i == 0), stop=(i == len(trip) - 1))
                return ins
            S.op("pe", router, reads=["wrh", "wrl"] + [("XH", c, tt) for c in range(C) for tt in range(NTT)]
                 + [("XL", c, tt) for c in range(C) for tt in range(NTT)], writes=[("ps", bl)])
            v3 = lambda ap: ap.rearrange("p (j e) -> p j e", e=NE)
            v4 = lambda ap: ap.rearrange("p (j g i) -> p j g i", g=4, i=4)
            g3 = lambda ap: ap[:, 0:NJ * 4].rearrange("p (j g) -> p j g", g=4)
            LG, MX, EX, M1, EQ, E2, M2, GS, GM, GK, TH, CMB = RT
            tt_op("dve", v3(LG), v3(PS[bl][:, 0:NJ * NE]), BR_t[:].unsqueeze(1).to_broadcast([128, NJ, NE]), ALU.add,
                  [("ps", bl), "br"], ["LG"])
            S.op("dve", lambda e: e.tensor_reduce(out=MX[:, 0:NJ], in_=v3(LG), axis=AX.X, op=ALU.max), reads=["LG"], writes=["MX"])
            tt_op("dve", v3(LG), v3(LG), MX[:, 0:NJ].unsqueeze(2).to_broadcast([128, NJ, NE]), ALU.subtract, ["LG", "MX"], ["LG"])
            act(EX, LG, AF.Exp, ["LG"], ["EX"])
            S.op("dve", lambda e: e.tensor_reduce(out=M1[:, 0:NJ * 4], in_=v4(EX), axis=AX.X, op=ALU.max), reads=["EX"], writes=["M1"])
            tt_op("dve", v4(EQ), v4(EX), g3(M1).unsqueeze(3).to_broadcast([128, NJ, 4, 4]), ALU.is_equal, ["EX", "M1"], ["EQ"])
            stt("dve", E2, EQ, -4.0, EX, ALU.mult, ALU.add, ["EQ", "EX"], ["E2"])
            S.op("dve", lambda e: e.tensor_reduce(out=M2[:, 0:NJ * 4], in_=v4(E2), axis=AX.X, op=ALU.max), reads=["E2"], writes=["M2"])
            tt_op("dve", GS[:, 0:NJ * 4], M1[:, 0:NJ * 4], M2[:, 0:NJ * 4], ALU.add, ["M1", "M2"], ["GS"])
            S.op("dve", lambda e: e.tensor_reduce(out=GM[:, 0:NJ], in_=g3(GS), axis=AX.X, op=ALU.max), reads=["GS"], writes=["GM"])
            tt_op("dve", g3(GK), g3(GS), GM[:, 0:NJ].unsqueeze(2).to_broadcast([128, NJ, 4]), ALU.is_equal, ["GS", "GM"], ["GK"])
            ts_op("dve", GS[:, 0:NJ * 4], GS[:, 0:NJ * 4], 1e-30, None, ALU.max, None, ["GS"], ["GS"])
            S.op("dve", lambda e: e.reciprocal(out=GS[:, 0:NJ * 4], in_=GS[:, 0:NJ * 4]), reads=["GS"], writes=["GS"])
            tt_op("dve", GK[:, 0:NJ * 4], GK[:, 0:NJ * 4], GS[:, 0:NJ * 4], ALU.mult, ["GK", "GS"], ["GK"])
            tt_op("dve", v4(TH), v4(EX), g3(M2).unsqueeze(3).to_broadcast([128, NJ, 4, 4]), ALU.is_ge, ["EX", "M2"], ["TH"])
            tt_op("dve", TH, TH, EX, ALU.mult, ["TH", "EX"], ["TH"])
            tt_op("dve", v4(CMB), v4(TH), g3(GK).unsqueeze(3).to_broadcast([128, NJ, 4, 4]), ALU.mult, ["TH", "GK"], ["CMB"])
            if debug and l == 0:
                S.dma("sp", "st", lambda e: e.dma_start(out=dbg_d["d_comb"], in_=CMB), reads=["CMB"])
            CMBH = EQ.bitcast(BF16)[:, 0:256]
            CMBL = E2.bitcast(BF16)[:, 0:256]
            S.op("act", lambda e: e.copy(out=CMBH, in_=CMB), reads=["CMB", "EQ"], writes=["EQ"])
            tt_op("dve", CMBL, CMB, CMBH, ALU.subtract, ["CMB", "EQ", "E2"], ["E2"])

            items = [(ex, tt) for ex in range(NE) for tt in range(NTT)]

            def moe_gu(it, l=l):
                ex, tt = items[it]
                sg, wg = wget(("wg", l, ex), w_g_d[l, ex].rearrange("(k p) n -> p k n", p=128), (8, 512))
                su, wu = wget(("wu", l, ex), w_u_d[l, ex].rearrange("(k p) n -> p k n", p=128), (8, 512))
                cb, cbn = CBb[it % 2], "cb%d" % (it % 2)
                hb, hbn = Hb[it % 2], "h%d" % (it % 2)
                b = nbank()

                def cbf(e, b=b, ex=ex, tt=tt):
                    ins = None
                    for jj in range(4):
                        j = tt * 4 + jj
                        col = j * NE + ex
                        e.matmul(PS[b][:, jj * 128:(jj + 1) * 128], CMBH[:, col:col + 1].to_broadcast([128, 128]), IDENTB_t[:],
                                 start=True, stop=False)
                        ins = e.matmul(PS[b][:, jj * 128:(jj + 1) * 128], CMBL[:, col:col + 1].to_broadcast([128, 128]), IDENTB_t[:],
                                       start=False, stop=True)
                    return ins
                S.op("pe", cbf, reads=["EQ", "E2", "identb"], writes=[("ps", b)])
                S.op("act", lambda e, b=b, cb=cb: e.copy(out=cb, in_=PS[b][:]), reads=[("ps", b)], writes=[cbn])
                for hc in range(4):
                    bg = nbank()
                    mm_group(PS[bg][:], [(wg[:, k, hc * 128:(hc + 1) * 128], XH[:, k, tsl(tt)]) for k in range(C)],
                             [("w", sg)] + [("XH", k, tt) for k in range(C)], bg)
                    bu = nbank()
                    mm_group(PS[bu][:], [(wu[:, k, hc * 128:(hc + 1) * 128], XH[:, k, tsl(tt)]) for k in range(C)],
                             [("w", su)] + [("XH", k, tt) for k in range(C)], bu)
                    m0 = MS[hc % 2]
                    m0n = MSn[hc % 2]
                    act(m0, PS[bg][:], AF.Silu, [("ps", bg)], [m0n])
                    tt_op("dve", m0, PS[bu][:], m0, ALU.mult, [("ps", bu), m0n], [m0n])
                    tt_op("dve", hb[:, hc, :], m0, cb, ALU.mult, [m0n, cbn], [(hbn, hc)])

            def moe_down(it, l=l):
                ex, tt = items[it]
                sd, wd = wget(("wd", l, ex), w_d_d[l, ex].rearrange("(k p) n -> p k n", p=128), (4, 1024))
                hb, hbn = Hb[it % 2], "h%d" % (it % 2)
                for oc in range(C):
                    b = nbank()
                    mm_group(PS[b][:], [(wd[:, hc, oc * 128:(oc + 1) * 128], hb[:, hc, :]) for hc in range(4)],
                             [("w", sd)] + [(hbn, hc) for hc in range(4)], b)
                    tt_op("dve", ACC[:, oc, tsl(tt)], PS[b][:], ACC[:, oc, tsl(tt)], ALU.add, [("ps", b), ("ACC", oc)], [("ACC", oc)])

            moe_gu(0)
            wget(("wd", l, 0), w_d_d[l, 0].rearrange("(k p) n -> p k n", p=128), (4, 1024))
            for c in range(C):
                tt_op("pool", ACC[:, c, :], XH[:, c, :], XL[:, c, :], ALU.add,
                      [("XH", c, tt) for tt in range(NTT)] + [("XL", c, tt) for tt in range(NTT)], [("ACC", c)])
                ts_op("pool", ACC[:, c, :], ACC[:, c, :], ALPHA, None, ALU.mult, None, [("ACC", c)], [("ACC", c)])
            for it in range(len(items)):
                if it + 1 < len(items):
                    moe_gu(it + 1)
                moe_down(it)

            last = (l == n_layers - 1)
            nrot[0] = 4

            def ln2_stats(tt):
                bm, bq = (4, 5) if tt % 2 == 0 else (6, 7)
                ln_mean([(ONESF_t[:], ACC[:, c, tsl(tt)]) for c in range(C)], ["consts"] + [("ACC", c) for c in range(C)], bm)
                for c in range(C):
                    ln_sq(ACC[:, c, tsl(tt)], [("ACC", c)], c, C, ONESF_t[:], [MS[4], MS[5]], bq, "msq")
                return bm, bq

            LT = [(scr(1536, TW), ["M2", "GS"]), (scr(2048, TW), ["GM", "GK"]), (scr(2560, TW), ["TH", "CMB"])]

            def m0(j):
                tt, c = divmod(j, C)
                if c == 0:
                    bm, bq = (4, 5) if tt % 2 == 0 else (6, 7)
                    if tt + 1 < NTT:
                        ln2_stats(tt + 1)
                    ln_finish(MS[0], MS[1], MS[2], MS[3], MSn, bm, bq)
                a, an = LT[j % 3]
                tt_op("dve", a, ACC[:, c, tsl(tt)], MS[1], ALU.mult, [("ACC", c), "ms1"], an)
                tt_op("dve", a, a, MS[2], ALU.subtract, an + ["ms2"], an)

            def m1(j, l=l):
                tt, c = divmod(j, C)
                a, an = LT[j % 3]
                if last:
                    act(ACC[:, c, tsl(tt)], a, AF.Identity, an + ["par"], [("ACCo", c, tt)],
                        bias=pcol(l, P_L2B + c), scale=pcol(l, P_L2G + c))
                else:
                    act(a, a, AF.Identity, an + ["par"], an, bias=pcol(l, P_L2B + c), scale=pcol(l, P_L2G + c))
                    S.op("act", lambda e: e.copy(out=XH[:, c, tsl(tt)], in_=a), reads=an, writes=[("XH", c, tt)])

            def m2(j):
                tt, c = divmod(j, C)
                a, an = LT[j % 3]
                if last:
                    S.dma("sp", "st", lambda e: e.dma_start(out=y_d[:, c * T + tt * TW:c * T + (tt + 1) * TW],
                                                            in_=ACC[:, c, tsl(tt)]), reads=[("ACCo", c, tt)])
                else:
                    tt_op("pool", XL[:, c, tsl(tt)], a, XH[:, c, tsl(tt)], ALU.subtract, an + [("XH", c, tt)], [("XL", c, tt)])

            HILO_ENG[0] = "pool"
            ln2_stats(0)
            mst = [m0, m1, m2]
            for step in range(NI + len(mst)):
                for k in reversed(range(len(mst))):
                    j = step - k
                    if 0 <= j < NI:
                        mst[k](j)
            nrot[0] = 6
            HILO_ENG[0] = "dve"
        S.final_wait("sp", ["st"])
        S.emit(E)
    return nc


def _pack_params(inp):
    P = np.zeros((128, DEPTH * NPL), np.float32)
    f = lambda a: np.ascontiguousarray(np.asarray(a, np.float32))
    for l in range(DEPTH):
        o = l * NPL
        P[:, o + P_BIN:o + P_BIN + 72] = f(inp["b_in"])[l].reshape(72, 128).T
        caw = f(inp["conv_a_w"])[l].reshape(31, 8, 128)
        P[:, o + P_CAW:o + P_CAW + 248] = caw.transpose(2, 0, 1).reshape(128, 248)
        P[:, o + P_CAB:o + P_CAB + 8] = f(inp["conv_a_b"])[l].reshape(8, 128).T
        P[:, o + P_LAG:o + P_LAG + 8] = f(inp["ln_a_g"])[l].reshape(8, 128).T
        P[:, o + P_LAB:o + P_LAB + 8] = f(inp["ln_a_b"])[l].reshape(8, 128).T
        cbw = f(inp["conv_b_w"])[l].reshape(3, 8, 128)
        P[:, o + P_CBW:o + P_CBW + 24] = cbw.transpose(2, 0, 1).reshape(128, 24)
        P[:, o + P_CS:o + P_CS + 8] = f(inp["c_scale"])[l].reshape(8, 128).T
        P[:, o + P_BO:o + P_BO + 8] = f(inp["b_o"])[l].reshape(8, 128).T
        P[:, o + P_L1G:o + P_L1G + 8] = f(inp["ln1_g"])[l].reshape(8, 128).T
        P[:, o + P_L1B:o + P_L1B + 8] = f(inp["ln1_b"])[l].reshape(8, 128).T
        P[:, o + P_L2G:o + P_L2G + 8] = f(inp["ln2_g"])[l].reshape(8, 128).T
        P[:, o + P_L2B:o + P_L2B + 8] = f(inp["ln2_b"])[l].reshape(8, 128).T
    return P


_NC_CACHE = {}


def make_in_maps(inputs, cores):
    f = lambda a: np.ascontiguousarray(np.asarray(a, np.float32))
    x = f(inputs["x"])
    shared = {
        "w_in": f(inputs["w_in"]), "w_a_out": f(inputs["w_a_out"]), "w_b_out": f(inputs["w_b_out"]),
        "w_c_group": f(inputs["w_c_group"]), "w_o": f(inputs["w_o"]),
        "w_exp_gate": f(inputs["w_exp_gate"]), "w_exp_up": f(inputs["w_exp_up"]), "w_exp_down": f(inputs["w_exp_down"]),
        "params": _pack_params(inputs),
        "wr": np.ascontiguousarray(f(inputs["w_router"]).reshape(8, 128, NE).transpose(1, 0, 2).reshape(128, 8 * NE)),
        "br": np.ascontiguousarray(np.broadcast_to(f(inputs["b_router"])[None, :], (128, NE))),
    }
    maps = []
    for b in cores:
        m = dict(shared)
        m["xT"] = np.ascontiguousarray(x[b].T)
        maps.append(m)
    return maps


def kernel(**inputs):
    n = 8
    if "nc" not in _NC_CACHE:
        _NC_CACHE["nc"] = build_program()
    nc = _NC_CACHE["nc"]
    in_maps = make_in_maps(inputs, list(range(n)))
    res = run_bass_kernel_spmd(nc, in_maps, core_ids=list(range(n)))
    out = np.empty((n, T, D), np.float32)
    for b in range(n):
        yT = np.asarray(res.results[b]["yT"]).reshape(128, C, T)
        out[b] = yT.transpose(2, 1, 0).reshape(T, D)
    return out
```

```python
import numpy as np
from contextlib import ExitStack
import concourse.bass as bass
import concourse.mybir as mybir
from concourse.bass_utils import run_bass_kernel_spmd

F32 = mybir.dt.float32
BF16 = mybir.dt.bfloat16
AF = mybir.ActivationFunctionType
ALU = mybir.AluOpType
AX = mybir.AxisListType

D = 1024
T = 2048
TW = 512
NTT = T // TW
C = 8
DEPTH = 2
NE = 16
DE = 512
IN_COLS = 9216
ALPHA = float((2 * DEPTH) ** 0.25)
EPS = 1e-5


def _bf16_round(v):
    u = np.array([v], np.float32).view(np.uint32)
    u = (u + np.uint32(0x7FFF) + ((u >> np.uint32(16)) & np.uint32(1))) & np.uint32(0xFFFF0000)
    return float(u.view(np.float32)[0])


A_HI = _bf16_round(ALPHA)
A_LO = _bf16_round(ALPHA - A_HI)
NSLOT = 4
STRICT_SAME_ENGINE = True

P_BIN = 0
P_CAW = 72
P_CAB = 320
P_LAG = 328
P_LAB = 336
P_CBW = 344
P_CS = 368
P_BO = 376
P_L1G = 384
P_L1B = 392
P_L2G = 400
P_L2B = 408
NPL = 416


class Sched:
    def __init__(self, nc):
        self.nc = nc
        self.streams = {e: [] for e in ("pe", "act", "dve", "pool", "sp")}
        self.cnt = {}
        self.res = {}
        self.waited = {e: {} for e in self.streams}
        self.sems = {}

    def _deps(self, stream, reads, writes):
        deps = {}

        def add(sk, v, raw):
            if sk == stream and not raw and not STRICT_SAME_ENGINE:
                return
            if deps.get(sk, 0) < v:
                deps[sk] = v

        for r in reads:
            st = self.res.get(r)
            if st and st[0]:
                add(st[0][0], st[0][1], True)
        for w in writes:
            st = self.res.get(w)
            if st:
                if st[0]:
                    add(st[0][0], st[0][1], False)
                for sk, v in st[1].items():
                    add(sk, v, False)
        out = []
        wd = self.waited[stream]
        for sk, v in deps.items():
            if wd.get(sk, 0) >= v:
                continue
            wd[sk] = v
            out.append((sk, v))
        return out

    def _commit(self, sk, val, reads, writes):
        for r in reads:
            st = self.res.setdefault(r, [None, {}])
            st[1][sk] = val
        for w in writes:
            self.res[w] = [(sk, val), {}]

    def op(self, eng, fn, reads=(), writes=()):
        reads = list(reads)
        writes = list(writes)
        waits = self._deps(eng, reads, writes)
        val = self.cnt.get(eng, 0) + 1
        self.cnt[eng] = val
        self._commit(eng, val, reads, writes)
        self.streams[eng].append((waits, fn, eng, 1))
        return val

    def dma(self, stream, semkey, fn, reads=(), writes=()):
        reads = list(reads)
        writes = list(writes)
        waits = self._deps(stream, reads, writes)
        val = self.cnt.get(semkey, 0) + 16
        self.cnt[semkey] = val
        self._commit(semkey, val, reads, writes)
        self.streams[stream].append((waits, fn, semkey, 16))
        return val

    def barrier(self):
        allw = [(sk, v) for sk, v in self.cnt.items() if v > 0]
        for st in self.streams:
            if st == "pool":
                continue
            wd = self.waited[st]
            waits = []
            for sk, v in allw:
                if wd.get(sk, 0) < v:
                    wd[sk] = v
                    waits.append((sk, v))
            if waits:
                self.streams[st].append((waits, None, None, 0))
        self.res = {k: v for k, v in self.res.items() if isinstance(k, tuple) and k[0] == "w"}

    def final_wait(self, stream, semkeys):
        waits = [(sk, self.cnt[sk]) for sk in semkeys if self.cnt.get(sk, 0) > 0]
        self.streams[stream].append((waits, None, None, 0))

    def emit(self, E):
        nc = self.nc
        for sk in self.cnt:
            self.sems[sk] = E(nc.semaphore("s_" + str(sk)))
        block = E(nc.Block())
        sems = self.sems
        waited_vals = {}
        for st in self.streams.values():
            for waits, fn, sk, inc in st:
                for wsk, wv in waits:
                    waited_vals.setdefault(wsk, set()).add(wv)

        def run(stream):
            def body(eng):
                pend = 0
                cum = 0
                ops = self.streams[stream]
                last_idx = max([i for i, o in enumerate(ops) if o[1] is not None and o[2] == stream], default=-1)
                for idx, (waits, fn, sk, inc) in enumerate(ops):
                    for wsk, wv in waits:
                        eng.wait_ge(sems[wsk], wv)
                    if fn is None:
                        continue
                    ins = fn(eng)
                    if sk != stream:
                        ins.then_inc(sems[sk], inc)
                        continue
                    pend += inc
                    cum += inc
                    if cum in waited_vals.get(sk, ()) or idx == last_idx:
                        ins.then_inc(sems[sk], pend)
                        pend = 0
            return body

        block.sync(run("sp"))
        block.tensor(run("pe"))
        block.scalar(run("act"))
        block.vector(run("dve"))
        block.gpsimd(run("pool"))


def build_program(n_layers=DEPTH, debug=False):
    nc = bass.Bass("TRN2", target_bir_lowering=False)
    dram = lambda name, shape, kind="ExternalInput", dt=F32: nc.dram_tensor(name, shape, dt, kind=kind).ap()
    xT_d = dram("xT", [D, T])
    w_in_d = dram("w_in", [DEPTH, D, IN_COLS])
    w_a_d = dram("w_a_out", [DEPTH, D, D])
    w_b_d = dram("w_b_out", [DEPTH, D, D])
    w_c_d = dram("w_c_group", [DEPTH, 4, 256, 256])
    w_o_d = dram("w_o", [DEPTH, D, D])
    w_g_d = dram("w_exp_gate", [DEPTH, NE, D, DE])
    w_u_d = dram("w_exp_up", [DEPTH, NE, D, DE])
    w_d_d = dram("w_exp_down", [DEPTH, NE, DE, D])
    par_d = dram("params", [128, DEPTH * NPL])
    wr_d = dram("wr", [128, C * NE])
    br_d = dram("br", [128, NE])
    y_d = dram("yT", [128, C * T], kind="ExternalOutput")
    dbg_d = {}
    if debug:
        dbg_d["d_xh"] = dram("d_xh", [128, C * T], kind="ExternalOutput", dt=BF16)
        dbg_d["d_xl"] = dram("d_xl", [128, C * T], kind="ExternalOutput", dt=BF16)
        dbg_d["d_comb"] = dram("d_comb", [128, 256], kind="ExternalOutput")

    S = Sched(nc)
    with ExitStack() as es:
        E = es.enter_context
        sb = lambda name, shape, dt: E(nc.sbuf_tensor(name, shape, dt))
        XH_t = sb("XH", [128, C, T], BF16)
        XL_t = sb("XL", [128, C, T], BF16)
        R64_t = sb("R64", [128, C * T], F32)
        WS_t = [sb("WS%d" % i, [128, 8 * 512], BF16) for i in range(NSLOT)]
        PAR_t = sb("PAR", [128, DEPTH * NPL], F32)
        WR_t = sb("WRF", [128, C * NE], F32)
        WRH_t = sb("WRH", [128, C * NE], BF16)
        WRL_t = sb("WRL", [128, C * NE], BF16)
        BR_t = sb("BR", [128, NE], F32)
        IDENT_t = sb("IDENT", [128, 128], F32)
        ONESB_t = sb("ONESB", [128, 128], BF16)
        IDENTB_t = sb("IDENTB", [128, 128], BF16)
        ALH_t = sb("ALH", [128, 128], BF16)
        ALL_t = sb("ALL", [128, 128], BF16)
        ONESF_t = sb("ONESF", [128, 128], F32)
        EPS_t = sb("EPSC", [128, 1], F32)
        INVC_t = sb("INVC", [128, 4 * 16], F32)
        SCRW = 10368
        SCR_t = sb("SCR", [128, SCRW], F32)
        PS = [E(nc.psum_tensor("ps%d" % i, [128, TW], F32)) for i in range(8)]

        XH = XH_t[:]
        XL = XL_t[:]
        ACC = R64_t[:].rearrange("p (c t) -> p c t", c=C)
        r64b = R64_t[:].bitcast(BF16).rearrange("p (h c t) -> p h c t", h=2, c=C)
        BIG1 = r64b[:, 0]
        MERGED = r64b[:, 1]
        PAR = PAR_t[:]
        INVC = INVC_t[:].rearrange("p (g t) -> p g t", g=4)
        WRH = WRH_t[:].rearrange("p (k e) -> p k e", k=C)
        WRL = WRL_t[:].rearrange("p (k e) -> p k e", k=C)

        PAD = 32
        TLEN = PAD + T

        def scr(off, n, dt=F32):
            if dt == F32:
                return SCR_t[:, off:off + n]
            return SCR_t[:, off:off + (n + 1) // 2].bitcast(BF16)

        o = 0
        TA = scr(o, TLEN); o += TLEN
        TB = scr(o, TLEN); APAD2 = [scr(o, TLEN, BF16), scr(o + TLEN // 2, TLEN, BF16)]; o += TLEN
        TC = scr(o, TLEN); o += TLEN
        GT2 = [scr(o + i * 256, TW, BF16) for i in range(2)]; o += 512
        SQ2 = [scr(o + i * 256, TW, BF16) for i in range(2)]; o += 512
        SM = [scr(o + i * TW, TW) for i in range(6)]; o += 6 * TW
        assert o <= SCRW, o
        KD = 11
        NKP = 31 - KD
        DG = [scr(3 * TLEN + 1024, NKP * 128, BF16), scr(3 * TLEN + 1024 + 1536, NKP * 128, BF16)]
        DGn = [["sm0", "sm1", "sm2"], ["sm3", "sm4", "sm5"]]
        o = 0
        RT = [scr(o + i * 256, 256) for i in range(12)]; o += 12 * 256
        CBb = [scr(o, TW, BF16), scr(o + TW // 2, TW, BF16)]; o += TW
        Hb = [scr(o + i * 1024, 4 * TW, BF16).rearrange("p (c t) -> p c t", c=4) for i in range(2)]; o += 2048
        MS = [scr(o + i * TW, TW) for i in range(6)]; o += 6 * TW
        assert o <= SCRW, o

        bank_ctr = [0]
        nrot = [6]

        def nbank():
            b = bank_ctr[0] % nrot[0]
            bank_ctr[0] += 1
            return b

        slot_ctr = [0]
        live = {}
        slot_key = [None] * NSLOT

        def wget(key, src_ap, shape3):
            if key in live:
                s = live[key]
            else:
                s = slot_ctr[0] % NSLOT
                slot_ctr[0] += 1
                if slot_key[s] is not None:
                    del live[slot_key[s]]
                slot_key[s] = key
                live[key] = s
                k, n = shape3
                view = WS_t[s][:, 0:k * n].rearrange("p (k n) -> p k n", k=k)
                S.dma("pool", "w%d" % s, lambda e, view=view, src=src_ap: e.dma_start(out=view, in_=src),
                      writes=[("w", s)])
            k, n = shape3
            return s, WS_t[s][:, 0:k * n].rearrange("p (k n) -> p k n", k=k)

        def win_piece(l, p):
            src = w_in_d[l, :, p * 512:(p + 1) * 512].rearrange("(k p) n -> p k n", p=128)
            return wget(("win", l, p), src, (8, 512))

        def sq_piece(name, dram_ap, l, p):
            src = dram_ap[l, :, p * 512:(p + 1) * 512].rearrange("(k p) n -> p k n", p=128)
            return wget((name, l, p), src, (8, 512))

        def mm_group(out_ap, pairs, reads, bank):
            def fn(e):
                n = len(pairs)
                ins = None
                for i, (lh, rh) in enumerate(pairs):
                    ins = e.matmul(out_ap, lh, rh, start=(i == 0), stop=(i == n - 1))
                return ins
            S.op("pe", fn, reads=reads, writes=[("ps", bank)])

        def tsl(tt):
            return slice(tt * TW, (tt + 1) * TW)

        def proj(l, j, tt):
            s, wv = win_piece(l, j // 4)
            jj = j % 4
            b = nbank()
            mm_group(PS[b][:], [(wv[:, k, jj * 128:(jj + 1) * 128], XH[:, k, tsl(tt)]) for k in range(C)],
                     [("w", s)] + [("XH", k, tt) for k in range(C)], b)
            return b

        def pcol(l, off):
            return PAR[:, l * NPL + off:l * NPL + off + 1]

        def act(out, in_, func, reads, writes, bias=None, scale=1.0):
            kw = {}
            if bias is not None:
                kw["bias"] = bias
            S.op("act", lambda e: e.activation(out=out, in_=in_, func=func, scale=scale, **kw), reads=reads, writes=writes)

        def tt_op(eng, out, in0, in1, op, reads, writes):
            S.op(eng, lambda e: e.tensor_tensor(out=out, in0=in0, in1=in1, op=op), reads=reads, writes=writes)

        def stt(eng, out, in0, scalar, in1, op0, op1, reads, writes):
            S.op(eng, lambda e: e.scalar_tensor_tensor(out=out, in0=in0, scalar=scalar, in1=in1, op0=op0, op1=op1),
                 reads=reads, writes=writes)

        def ts_op(eng, out, in0, s1, s2, op0, op1, reads, writes):
            if s2 is None:
                S.op(eng, lambda e: e.tensor_scalar(out=out, in0=in0, scalar1=s1, scalar2=None, op0=op0), reads=reads, writes=writes)
            else:
                S.op(eng, lambda e: e.tensor_scalar(out=out, in0=in0, scalar1=s1, scalar2=s2, op0=op0, op1=op1),
                     reads=reads, writes=writes)

        BM, BQ = 6, 7

        def ln_mean(pairs, reads, bm=BM):
            mm_group(PS[bm][:], pairs, reads, bm)

        def ln_sq(src, reads, i, n, ones, sq, bq=BQ, sqn="sqt"):
            q = sq[i % 2]
            qn = sqn + str(i % 2)
            act(q, src, AF.Square, reads, [qn])
            S.op("pe", lambda e: e.matmul(PS[bq][:], ones, q, start=(i == 0), stop=(i == n - 1)),
                 reads=["consts", qn], writes=[("ps", bq)])

        def ln_finish(sm_mean, sm_rstd, sm_mr, sm_tmp, names, bm=BM, bq=BQ):
            nm, nr, nmr, nt = names[0], names[1], names[2], names[3]
            act(sm_mean, PS[bm][:], AF.Copy, [("ps", bm)], [nm])
            tt_op("dve", sm_tmp, sm_mean, sm_mean, ALU.mult, [nm], [nt])
            tt_op("dve", sm_tmp, PS[bq][:], sm_tmp, ALU.subtract, [("ps", bq), nt], [nt])
            ts_op("dve", sm_tmp, sm_tmp, 0.0, None, ALU.max, None, [nt], [nt])
            act(sm_rstd, sm_tmp, AF.Sqrt, [nt, "consts"], [nr], bias=EPS_t[:, 0:1])
            S.op("dve", lambda e: e.reciprocal(out=sm_rstd, in_=sm_rstd), reads=[nr], writes=[nr])
            tt_op("dve", sm_mr, sm_mean, sm_rstd, ALU.mult, [nm, nr], [nmr])

        HILO_ENG = ["dve"]

        def split_hilo(src_f32, c, tt, src_res):
            S.op("act", lambda e: e.copy(out=XH[:, c, tsl(tt)], in_=src_f32), reads=[src_res], writes=[("XH", c, tt)])
            tt_op(HILO_ENG[0], XL[:, c, tsl(tt)], src_f32, XH[:, c, tsl(tt)], ALU.subtract, [src_res, ("XH", c, tt)], [("XL", c, tt)])

        S.dma("sp", "ld0", lambda e: e.dma_start(out=PAR, in_=par_d), writes=["par"])
        S.dma("sp", "ld1", lambda e: e.dma_start(out=WR_t[:], in_=wr_d), writes=["wrf"])
        S.dma("sp", "ld2", lambda e: e.dma_start(out=BR_t[:], in_=br_d), writes=["br"])

        S.op("pool", lambda e: e.memset(IDENT_t[:], 0.0), writes=["ident0"])
        S.op("pool", lambda e: e.affine_select(out=IDENT_t[:], in_=IDENT_t[:], compare_op=ALU.not_equal, fill=1.0, base=0,
                                               pattern=[[-1, 128]], channel_multiplier=1), reads=["ident0"], writes=["ident0"])

        def consts(e):
            e.memset(ONESB_t[:], 1.0 / D)
            e.memset(ONESF_t[:], 1.0 / D)
            e.memset(EPS_t[:], EPS)
            for g, w in enumerate((2, 4, 8, 16)):
                e.memset(INVC[:, g, w - 1:16], 1.0 / w)
                for t in range(w - 1):
                    e.memset(INVC[:, g, t:t + 1], 1.0 / (t + 1))
            return e.memset(SCR_t[:], 0.0)
        S.op("pool", consts, reads=["ident0"], writes=["consts", "scrzero"])
        S.op("dve", lambda e: e.tensor_copy(out=IDENTB_t[:], in_=IDENT_t[:]), reads=["consts"], writes=["identb"])
        S.op("dve", lambda e: e.tensor_scalar(out=ALH_t[:], in0=IDENTB_t[:], scalar1=A_HI, scalar2=None, op0=ALU.mult),
             reads=["identb"], writes=["alh"])
        S.op("dve", lambda e: e.tensor_scalar(out=ALL_t[:], in0=IDENTB_t[:], scalar1=A_LO, scalar2=None, op0=ALU.mult),
             reads=["identb"], writes=["all"])
        S.op("act", lambda e: e.copy(out=WRH_t[:], in_=WR_t[:]), reads=["wrf"], writes=["wrh"])
        tt_op("dve", WRL_t[:], WR_t[:], WRH_t[:], ALU.subtract, ["wrf", "wrh"], ["wrl"])

        XST = R64_t[:].rearrange("p (b k t) -> p b k t", b=NTT, k=C)
        xsrc = xT_d.rearrange("(k p) t -> p k t", p=128)
        for tt in range(NTT):
            rk = "xin%d" % tt
            S.dma("sp", "ldx%d" % tt, lambda e, tt=tt: e.dma_start(out=XST[:, tt], in_=xsrc[:, :, tsl(tt)]), writes=[rk])
            S.op("act", lambda e, tt=tt: e.copy(out=XH[:, :, tsl(tt)], in_=XST[:, tt]), reads=[rk],
                 writes=[("XH", c, tt) for c in range(C)] + ["xconv"])
            tt_op("dve", XL[:, :, tsl(tt)], XST[:, tt], XH[:, :, tsl(tt)], ALU.subtract, [rk] + [("XH", c, tt) for c in range(C)],
                  [("XL", c, tt) for c in range(C)] + ["xconv"])

        SMn = ["sm%d" % i for i in range(6)]
        MSn = ["ms%d" % i for i in range(6)]
        git = [0]

        def gate(l, j, tt):
            i = git[0] % 2
            git[0] += 1
            bg = proj(l, j, tt)
            act(GT2[i], PS[bg][:], AF.Sigmoid, [("ps", bg), "par"], ["gt%d" % i], bias=pcol(l, P_BIN + j))
            return GT2[i], "gt%d" % i

        def pslice(buf, tt):
            return buf[:, PAD + tt * TW:PAD + (tt + 1) * TW]

        for l in range(n_layers):
            if l > 0:
                S.barrier()
                S.op("dve", lambda e: e.memset(SCR_t[:, 0:3 * TLEN], 0.0), writes=["TA", "TB", "TC"] + ["ap%d_%d" % (i, t) for i in range(2) for t in range(NTT)] + ["ta%d" % t for t in range(NTT)])

            nrot[0] = 4
            for c in range(C):
                dg, dgn = DG[c % 2], DGn[c % 2]
                APAD = APAD2[c % 2]
                apn = ["ap%d_%d" % (c % 2, t) for t in range(NTT)]

                def mkdiag(e, c=c, dg=dg, l=l):
                    ins = None
                    for k in range(KD, 31):
                        ins = e.activation(out=dg[:, (k - KD) * 128:(k - KD + 1) * 128], in_=IDENTB_t[:], func=AF.Copy,
                                           scale=pcol(l, P_CAW + k * 8 + c))
                    return ins
                S.op("act", mkdiag, reads=["identb", "par"], writes=dgn)
                x0 = ["scrzero"] if l == 0 else []
                for tt in range(NTT):
                    b = proj(l, c, tt)
                    act(pslice(TA, tt), PS[b][:], AF.Identity, [("ps", b), "par"] + x0, ["ta%d" % tt], bias=pcol(l, P_BIN + c))
                    b = proj(l, 8 + c, tt)
                    act(pslice(APAD, tt), PS[b][:], AF.Sigmoid, [("ps", b), "par"] + x0, [apn[tt]], bias=pcol(l, P_BIN + 8 + c))
                    tt_op("dve", pslice(APAD, tt), pslice(TA, tt), pslice(APAD, tt), ALU.mult, ["ta%d" % tt, apn[tt]], [apn[tt]])
                cb_ = []
                for tt in range(NTT):
                    b = 4 + tt
                    cb_.append(b)
                    mm_group(PS[b][:], [(dg[:, (k - KD) * 128:(k - KD + 1) * 128],
                                         APAD[:, PAD - 30 + k + tt * TW:PAD - 30 + k + (tt + 1) * TW]) for k in range(KD, 31)],
                             [apn[tt]] + ([apn[tt - 1]] if tt > 0 else []) + dgn, b)
                apall = list(apn)
                ts_op("dve", TC[:, PAD:], APAD[:, PAD - 30:PAD - 30 + T], pcol(l, P_CAW + 0 * 8 + c), pcol(l, P_CAB + c),
                      ALU.mult, ALU.add, apall + ["par"] + x0, ["TC"])
                for k in range(1, KD):
                    stt("dve", TC[:, PAD:], APAD[:, PAD - 30 + k:PAD - 30 + k + T], pcol(l, P_CAW + k * 8 + c), TC[:, PAD:],
                        ALU.mult, ALU.add, apall + ["TC", "par"], ["TC"])
                for tt in range(NTT):
                    b = cb_[tt]
                    tt_op("dve", BIG1[:, c, tsl(tt)], PS[b][:], pslice(TC, tt), ALU.add,
                          [("ps", b), "TC"] + (["xconv"] if l == 0 else []), [("BIG1", c, tt)])
            nrot[0] = 6
            wa = [sq_piece("wa", w_a_d, l, p) for p in range(2)]

            RS = [(pslice(TA, 0), "ta0"), (pslice(TA, 1), "ta1")]
            MR = [(pslice(TA, 2), "ta2"), (pslice(TA, 3), "ta3")]

            def st_mean(tt):
                ln_mean([(ONESB_t[:], BIG1[:, c, tsl(tt)]) for c in range(C)], ["consts"] + [("BIG1", c, tt) for c in range(C)])

            def st_sq(tt, c):
                q, qn = SQ2[c % 2], "sqt%d" % (c % 2)
                act(q, BIG1[:, c, tsl(tt)], AF.Square, [("BIG1", c, tt)], [qn])

            def st_sqmm(tt, c):
                q, qn = SQ2[c % 2], "sqt%d" % (c % 2)
                S.op("pe", lambda e: e.matmul(PS[BQ][:], ONESB_t[:], q, start=(c == 0), stop=(c == C - 1)),
                     reads=["consts", qn], writes=[("ps", BQ)])

            def st_fin(tt):
                (r, rn), (m, mn) = RS[tt % 2], MR[tt % 2]
                ln_finish(SM[0], r, m, SM[3], ["sm0", rn, mn, "sm3"])

            def lna_stats(tt):
                st_mean(tt)
                for c in range(C):
                    st_sq(tt, c)
                    st_sqmm(tt, c)
                st_fin(tt)

            NA = [(SM[4], "sm4"), (SM[5], "sm5")]
            abank = {}

            def nn0(tt, c):
                a, an = NA[c % 2]
                (r, rn), (m, mn) = RS[tt % 2], MR[tt % 2]
                tt_op("dve", a, BIG1[:, c, tsl(tt)], r, ALU.mult, [("BIG1", c, tt), rn], [an])
                tt_op("dve", a, a, m, ALU.subtract, [an, mn], [an])

            def nn1(tt, c, l=l):
                a, an = NA[c % 2]
                act(BIG1[:, c, tsl(tt)], a, AF.Silu, [an, "par"], [("BIG1", c, tt)],
                    bias=pcol(l, P_LAB + c), scale=pcol(l, P_LAG + c))

            def aa0(tt, oc, l=l):
                s_, wv = wa[oc // 4]
                bg = proj(l, 48 + oc, tt)
                b = nbank()
                oo = oc % 4
                mm_group(PS[b][:], [(wv[:, k, oo * 128:(oo + 1) * 128], BIG1[:, k, tsl(tt)]) for k in range(C)],
                         [("w", s_)] + [("BIG1", k, tt) for k in range(C)], b)
                abank[(tt, oc)] = (bg, b)

            def aa1(tt, oc, l=l):
                bg, b = abank[(tt, oc)]
                i = oc % 2
                act(GT2[i], PS[bg][:], AF.Sigmoid, [("ps", bg), "par"], ["gt%d" % i], bias=pcol(l, P_BIN + 48 + oc))

            def aa2(tt, oc):
                bg, b = abank[(tt, oc)]
                i = oc % 2
                tt_op("dve", MERGED[:, oc, tsl(tt)], PS[b][:], GT2[i], ALU.mult, [("ps", b), "gt%d" % i], [("MG", oc, tt)])

            lna_stats(0)
            for step in range(C + 1):
                if 0 <= step - 1 < C:
                    nn1(0, step - 1)
                if step < C:
                    nn0(0, step)
            lna_stats(1)
            for tt in range(NTT):
                nxt = tt + 1 < NTT
                nx2 = tt + 2 < NTT
                for step in range(C + 2):
                    if 0 <= step - 2 < C:
                        aa2(tt, step - 2)
                    if 0 <= step - 1 < C:
                        aa1(tt, step - 1)
                        if nxt:
                            nn1(tt + 1, step - 1)
                        if nx2:
                            st_sqmm(tt + 2, step - 1)
                    if step < C:
                        if nxt:
                            nn0(tt + 1, step)
                        if nx2:
                            if step == 0:
                                st_mean(tt + 2)
                            st_sq(tt + 2, step)
                        aa0(tt, step)
                if nx2:
                    st_fin(tt + 2)

            S.op("dve", lambda e: e.memset(SM[5][:, 0:1], 0.0), writes=["TA", "TB", "sm5"] + ["ap%d_%d" % (i, t) for i in range(2) for t in range(NTT)] + ["ta%d" % t for t in range(NTT)])
            for c in range(C):
                for tt in range(NTT):
                    b = proj(l, 24 + c, tt)
                    act(pslice(TA, tt), PS[b][:], AF.Identity, [("ps", b), "par"], ["TA"], bias=pcol(l, P_BIN + 24 + c))
                    b = proj(l, 32 + c, tt)
                    stt("dve", pslice(TB, tt), PS[b][:], pcol(l, P_BIN + 32 + c), pslice(TA, tt), ALU.add, ALU.mult,
                        [("ps", b), "TA", "par"], ["TB"])
                ts_op("dve", TC[:, PAD:], TB[:, PAD - 2:PAD - 2 + T], pcol(l, P_CBW + 0 * 8 + c), None, ALU.mult, None,
                      ["TB", "par"], ["TC"])
                for k in (1, 2):
                    stt("dve", TC[:, PAD:], TB[:, PAD - 2 + k:PAD - 2 + k + T], pcol(l, P_CBW + k * 8 + c), TC[:, PAD:],
                        ALU.mult, ALU.add, ["TB", "TC", "par"], ["TC"])
                for tt in range(NTT):
                    b = proj(l, 16 + c, tt)
                    stt("dve", BIG1[:, c, tsl(tt)], PS[b][:], pcol(l, P_BIN + 16 + c), pslice(TC, tt),
                        ALU.add, ALU.mult, [("ps", b), "TC", "par"], [("BIG1", c, tt)])
            for oc in range(C):
                s, wv = sq_piece("wb", w_b_d, l, oc // 4)
                for tt in range(NTT):
                    gt, gtn = gate(l, 56 + oc, tt)
                    b = nbank()
                    oo = oc % 4
                    mm_group(PS[b][:], [(wv[:, k, oo * 128:(oo + 1) * 128], BIG1[:, k, tsl(tt)]) for k in range(C)],
                             [("w", s)] + [("BIG1", k, tt) for k in range(C)], b)
                    tt_op("dve", SM[3], PS[b][:], gt, ALU.mult, [("ps", b), gtn], ["sm3"])
                    tt_op("dve", MERGED[:, oc, tsl(tt)], MERGED[:, oc, tsl(tt)], SM[3], ALU.add, [("MG", oc, tt), "sm3"], [("MG", oc, tt)])

            for g, w in enumerate((2, 4, 8, 16)):
                for c in (2 * g, 2 * g + 1):
                    for tt in range(NTT):
                        b = proj(l, 40 + c, tt)
                        act(pslice(TA, tt), PS[b][:], AF.Identity, [("ps", b), "par"], ["TA"], bias=pcol(l, P_BIN + 40 + c))
                    src, srcn = TA, "TA"
                    bufs = [(TB, "TB"), (TC, "TC")]
                    sh = 1
                    i = 0
                    while sh < w:
                        dst, dstn = bufs[i % 2]
                        tt_op("dve", dst[:, PAD:], src[:, PAD:], src[:, PAD - sh:PAD - sh + T], ALU.add, [srcn], [dstn])
                        src, srcn = dst, dstn
                        sh *= 2
                        i += 1
                    stt("dve", BIG1[:, c, :], src[:, PAD:], 1.0 / w, TA[:, PAD:], ALU.mult, ALU.subtract, [srcn, "TA"], [("BIG1", c, t4) for t4 in range(NTT)])
                    tt_op("dve", SM[3][:, 0:16], src[:, PAD:PAD + 16], INVC[:, g, :], ALU.mult, [srcn, "consts"], ["sm3"])
                    tt_op("dve", BIG1[:, c, 0:16], SM[3][:, 0:16], TA[:, PAD:PAD + 16], ALU.subtract, ["sm3", "TA", ("BIG1", c, 0)], [("BIG1", c, 0)])
            srcw = w_c_d[l].rearrange("g (k p) n -> p (g k) n", p=128)
            s, wv = wget(("wc", l), srcw, (8, 256))
            for oc in range(C):
                g = oc // 2
                for tt in range(NTT):
                    gt, gtn = gate(l, 64 + oc, tt)
                    b = nbank()
                    oo = oc % 2
                    mm_group(PS[b][:], [(wv[:, 2 * g + k, oo * 128:(oo + 1) * 128], BIG1[:, 2 * g + k, tsl(tt)]) for k in range(2)],
                             [("w", s)] + [("BIG1", 2 * g + k, tt) for k in range(2)], b)
                    stt("dve", SM[3], PS[b][:], pcol(l, P_CS + oc), gt, ALU.mult, ALU.mult, [("ps", b), gtn, "par"], ["sm3"])
                    tt_op("dve", MERGED[:, oc, tsl(tt)], MERGED[:, oc, tsl(tt)], SM[3], ALU.add, [("MG", oc, tt), "sm3"], [("MG", oc, tt)])

            so0, wo0 = sq_piece("wo", w_o_d, l, 0)
            so1, wo1 = sq_piece("wo", w_o_d, l, 1)
            nrot[0] = 4
            HILO_ENG[0] = "pool"

            VT = [(TA[:, i * TW:(i + 1) * TW], "va%d" % i) for i in range(4)] + [(TC[:, i * TW:(i + 1) * TW], "vc%d" % i) for i in range(4)]
            NT = [(TB[:, i * TW:(i + 1) * TW], "na%d" % i) for i in range(4)] + [(SM[4], "sm4"), (SM[5], "sm5")]
            SQ4 = [(SQ2[0], "sqt0"), (SQ2[1], "sqt1"), (GT2[0], "gt0"), (GT2[1], "gt1")]
            NI = NTT * C
            obank = {}

            def stat_banks(tt):
                return (4, 5) if tt % 2 == 0 else (6, 7)

            def c0(i, l=l):
                tt, oc = divmod(i, C)
                s, wv = (so0, wo0) if oc < 4 else (so1, wo1)
                oo = oc % 4
                b = nbank()
                obank[i] = b
                mm_group(PS[b][:], [(wv[:, k, oo * 128:(oo + 1) * 128], MERGED[:, k, tsl(tt)]) for k in range(C)]
                         + [(ALH_t[:], XH[:, oc, tsl(tt)]), (ALH_t[:], XL[:, oc, tsl(tt)]), (ALL_t[:], XH[:, oc, tsl(tt)])],
                         [("w", s), "alh", "all", ("XH", oc, tt), ("XL", oc, tt)] + [("MG", k, tt) for k in range(C)], b)

            def c1(i, l=l):
                tt, oc = divmod(i, C)
                v, vn = VT[i % 8]
                extra = (["TA"] if i < 4 else ["TC"]) if i < 8 else []
                b = obank[i]
                act(v, PS[b][:], AF.Identity, [("ps", b), "par"], [vn] + extra, bias=pcol(l, P_BO + oc))

            def c2(i):
                pass

            def c3(i):
                tt, oc = divmod(i, C)
                v, vn = VT[i % 8]
                q, qn = SQ4[i % 4]
                act(q, v, AF.Square, [vn], [qn])
                S.op("act", lambda e: e.copy(out=XH[:, oc, tsl(tt)], in_=v), reads=[vn], writes=[("XH", oc, tt)])

            def c4(i):
                tt, oc = divmod(i, C)
                v, vn = VT[i % 8]
                q, qn = SQ4[i % 4]
                bm, bq = stat_banks(tt)
                tt_op("pool", XL[:, oc, tsl(tt)], v, XH[:, oc, tsl(tt)], ALU.subtract, [vn, ("XH", oc, tt)], [("XL", oc, tt)])
                S.op("pe", lambda e: e.matmul(PS[bq][:], ONESB_t[:], q, start=(oc == 0), stop=(oc == C - 1)),
                     reads=["consts", qn], writes=[("ps", bq)])

            def c5(i):
                tt, oc = divmod(i, C)
                if oc == C - 1:
                    bm, bq = stat_banks(tt)
                    ln_mean([(ONESB_t[:], XH[:, c, tsl(tt)]) for c in range(C)] + [(ONESB_t[:], XL[:, c, tsl(tt)]) for c in range(C)],
                            ["consts"] + [("XH", c, tt) for c in range(C)] + [("XL", c, tt) for c in range(C)], bm)

            def n0(j):
                tt, c = divmod(j, C)
                if c == 0:
                    bm, bq = stat_banks(tt)
                    ln_finish(SM[0], SM[1], SM[2], SM[3], SMn, bm, bq)
                a, an = NT[j % 6]
                extra = ["TB"] if j < 4 else []
                tt_op("pool", a, XH[:, c, tsl(tt)], XL[:, c, tsl(tt)], ALU.add, [("XH", c, tt), ("XL", c, tt)], [an] + extra)

            def n1(j):
                a, an = NT[j % 6]
                tt_op("dve", a, a, SM[1], ALU.mult, [an, "sm1"], [an])
                tt_op("dve", a, a, SM[2], ALU.subtract, [an, "sm2"], [an])

            def n2(j, l=l):
                tt, c = divmod(j, C)
                a, an = NT[j % 6]
                act(a, a, AF.Identity, [an, "par"], [an], bias=pcol(l, P_L1B + c), scale=pcol(l, P_L1G + c))
                S.op("act", lambda e: e.copy(out=XH[:, c, tsl(tt)], in_=a), reads=[an], writes=[("XH", c, tt)])

            def n3(j):
                tt, c = divmod(j, C)
                a, an = NT[j % 6]
                tt_op("pool", XL[:, c, tsl(tt)], a, XH[:, c, tsl(tt)], ALU.subtract, [an, ("XH", c, tt)], [("XL", c, tt)])

            cst = [c0, c1, c2, c3, c4, c5]
            nst = [n0, n1, n2, n3]
            OFF = C + len(cst)
            for step in range(NI + OFF + len(nst)):
                for k in reversed(range(len(nst))):
                    j = step - OFF - k
                    if 0 <= j < NI:
                        nst[k](j)
                for k in reversed(range(len(cst))):
                    i = step - k
                    if 0 <= i < NI:
                        cst[k](i)
            nrot[0] = 6
            HILO_ENG[0] = "dve"

            if debug and l == 0:
                S.dma("sp", "st", lambda e: e.dma_start(out=dbg_d["d_xh"], in_=XH_t[:].rearrange("p c t -> p (c t)")),
                      reads=[("XH", c, tt) for c in range(C) for tt in range(NTT)])
                S.dma("sp", "st", lambda e: e.dma_start(out=dbg_d["d_xl"], in_=XL_t[:].rearrange("p c t -> p (c t)")),
                      reads=[("XL", c, tt) for c in range(C) for tt in range(NTT)])

            S.barrier()
            bl = nbank()
            NJ = T // 128

            def router(e, bl=bl):
                ins = None
                for j in range(NJ):
                    trip = []
                    for k in range(C):
                        trip.append((XH[:, k, j * 128:(j + 1) * 128], WRH[:, k, :]))
                        trip.append((XL[:, k, j * 128:(j + 1) * 128], WRH[:, k, :]))
                        trip.append((XH[:, k, j * 128:(j + 1) * 128], WRL[:, k, :]))
                    for i, (lh, rh) in enumerate(trip):
                        ins = e.matmul(PS[bl][:, j * NE:(j + 1) * NE], lh, rh, start=(i == 0), stop=(i == len(trip) - 1))
                return ins
            S.op("pe", router, reads=["wrh", "wrl"] + [("XH", c, tt) for c in range(C) for tt in range(NTT)]
                 + [("XL", c, tt) for c in range(C) for tt in range(NTT)], writes=[("ps", bl)])
            v3 = lambda ap: ap.rearrange("p (j e) -> p j e", e=NE)
            v4 = lambda ap: ap.rearrange("p (j g i) -> p j g i", g=4, i=4)
            g3 = lambda ap: ap[:, 0:NJ * 4].rearrange("p (j g) -> p j g", g=4)
            LG, MX, EX, M1, EQ, E2, M2, GS, GM, GK, TH, CMB = RT
            tt_op("dve", v3(LG), v3(PS[bl][:, 0:NJ * NE]), BR_t[:].unsqueeze(1).to_broadcast([128, NJ, NE]), ALU.add,
                  [("ps", bl), "br"], ["LG"])
            S.op("dve", lambda e: e.tensor_reduce(out=MX[:, 0:NJ], in_=v3(LG), axis=AX.X, op=ALU.max), reads=["LG"], writes=["MX"])
            tt_op("dve", v3(LG), v3(LG), MX[:, 0:NJ].unsqueeze(2).to_broadcast([128, NJ, NE]), ALU.subtract, ["LG", "MX"], ["LG"])
            act(EX, LG, AF.Exp, ["LG"], ["EX"])
            S.op("dve", lambda e: e.tensor_reduce(out=M1[:, 0:NJ * 4], in_=v4(EX), axis=AX.X, op=ALU.max), reads=["EX"], writes=["M1"])
            tt_op("dve", v4(EQ), v4(EX), g3(M1).unsqueeze(3).to_broadcast([128, NJ, 4, 4]), ALU.is_equal, ["EX", "M1"], ["EQ"])
            stt("dve", E2, EQ, -4.0, EX, ALU.mult, ALU.add, ["EQ", "EX"], ["E2"])
            S.op("dve", lambda e: e.tensor_reduce(out=M2[:, 0:NJ * 4], in_=v4(E2), axis=AX.X, op=ALU.max), reads=["E2"], writes=["M2"])
            tt_op("dve", GS[:, 0:NJ * 4], M1[:, 0:NJ * 4], M2[:, 0:NJ * 4], ALU.add, ["M1", "M2"], ["GS"])
            S.op("dve", lambda e: e.tensor_reduce(out=GM[:, 0:NJ], in_=g3(GS), axis=AX.X, op=ALU.max), reads=["GS"], writes=["GM"])
            tt_op("dve", g3(GK), g3(GS), GM[:, 0:NJ].unsqueeze(2).to_broadcast([128, NJ, 4]), ALU.is_equal, ["GS", "GM"], ["GK"])
            ts_op("dve", GS[:, 0:NJ * 4], GS[:, 0:NJ * 4], 1e-30, None, ALU.max, None, ["GS"], ["GS"])
            S.op("dve", lambda e: e.reciprocal(out=GS[:, 0:NJ * 4], in_=GS[:, 0:NJ * 4]), reads=["GS"], writes=["GS"])
            tt_op("dve", GK[:, 0:NJ * 4], GK[:, 0:NJ * 4], GS[:, 0:NJ * 4], ALU.mult, ["GK", "GS"], ["GK"])
            tt_op("dve", v4(TH), v4(EX), g3(M2).unsqueeze(3).to_broadcast([128, NJ, 4, 4]), ALU.is_ge, ["EX", "M2"], ["TH"])
            tt_op("dve", TH, TH, EX, ALU.mult, ["TH", "EX"], ["TH"])
            tt_op("dve", v4(CMB), v4(TH), g3(GK).unsqueeze(3).to_broadcast([128, NJ, 4, 4]), ALU.mult, ["TH", "GK"], ["CMB"])
            if debug and l == 0:
                S.dma("sp", "st", lambda e: e.dma_start(out=dbg_d["d_comb"], in_=CMB), reads=["CMB"])
            CMBH = EQ.bitcast(BF16)[:, 0:256]
            CMBL = E2.bitcast(BF16)[:, 0:256]
            S.op("act", lambda e: e.copy(out=CMBH, in_=CMB), reads=["CMB", "EQ"], writes=["EQ"])
            tt_op("dve", CMBL, CMB, CMBH, ALU.subtract, ["CMB", "EQ", "E2"], ["E2"])

            items = [(ex, tt) for ex in range(NE) for tt in range(NTT)]

            def moe_gu(it, l=l):
                ex, tt = items[it]
                sg, wg = wget(("wg", l, ex), w_g_d[l, ex].rearrange("(k p) n -> p k n", p=128), (8, 512))
                su, wu = wget(("wu", l, ex), w_u_d[l, ex].rearrange("(k p) n -> p k n", p=128), (8, 512))
                cb, cbn = CBb[it % 2], "cb%d" % (it % 2)
                hb, hbn = Hb[it % 2], "h%d" % (it % 2)
                b = nbank()

                def cbf(e, b=b, ex=ex, tt=tt):
                    ins = None
                    for jj in range(4):
                        j = tt * 4 + jj
                        col = j * NE + ex
                        e.matmul(PS[b][:, jj * 128:(jj + 1) * 128], CMBH[:, col:col + 1].to_broadcast([128, 128]), IDENTB_t[:],
                                 start=True, stop=False)
                        ins = e.matmul(PS[b][:, jj * 128:(jj + 1) * 128], CMBL[:, col:col + 1].to_broadcast([128, 128]), IDENTB_t[:],
                                       start=False, stop=True)
                    return ins
                S.op("pe", cbf, reads=["EQ", "E2", "identb"], writes=[("ps", b)])
                S.op("act", lambda e, b=b, cb=cb: e.copy(out=cb, in_=PS[b][:]), reads=[("ps", b)], writes=[cbn])
                for hc in range(4):
                    bg = nbank()
                    mm_group(PS[bg][:], [(wg[:, k, hc * 128:(hc + 1) * 128], XH[:, k, tsl(tt)]) for k in range(C)],
                             [("w", sg)] + [("XH", k, tt) for k in range(C)], bg)
                    bu = nbank()
                    mm_group(PS[bu][:], [(wu[:, k, hc * 128:(hc + 1) * 128], XH[:, k, tsl(tt)]) for k in range(C)],
                             [("w", su)] + [("XH", k, tt) for k in range(C)], bu)
                    m0 = MS[hc % 2]
                    m0n = MSn[hc % 2]
                    act(m0, PS[bg][:], AF.Silu, [("ps", bg)], [m0n])
                    tt_op("dve", m0, PS[bu][:], m0, ALU.mult, [("ps", bu), m0n], [m0n])
                    tt_op("dve", hb[:, hc, :], m0, cb, ALU.mult, [m0n, cbn], [(hbn, hc)])

            def moe_down(it, l=l):
                ex, tt = items[it]
                sd, wd = wget(("wd", l, ex), w_d_d[l, ex].rearrange("(k p) n -> p k n", p=128), (4, 1024))
                hb, hbn = Hb[it % 2], "h%d" % (it % 2)
                for oc in range(C):
                    b = nbank()
                    mm_group(PS[b][:], [(wd[:, hc, oc * 128:(oc + 1) * 128], hb[:, hc, :]) for hc in range(4)],
                             [("w", sd)] + [(hbn, hc) for hc in range(4)], b)
                    tt_op("dve", ACC[:, oc, tsl(tt)], PS[b][:], ACC[:, oc, tsl(tt)], ALU.add, [("ps", b), ("ACC", oc)], [("ACC", oc)])

            moe_gu(0)
            for c in range(C):
                ts_op("dve", ACC[:, c, :], XH[:, c, :], ALPHA, None, ALU.mult, None, [("XH", c, tt) for tt in range(NTT)], [("ACC", c)])
                stt("dve", ACC[:, c, :], XL[:, c, :], ALPHA, ACC[:, c, :], ALU.mult, ALU.add,
                    [("XL", c, tt) for tt in range(NTT)] + [("ACC", c)], [("ACC", c)])
            for it in range(len(items)):
                if it + 1 < len(items):
                    moe_gu(it + 1)
                moe_down(it)

            last = (l == n_layers - 1)
            nrot[0] = 4

            def ln2_stats(tt):
                bm, bq = (4, 5) if tt % 2 == 0 else (6, 7)
                ln_mean([(ONESF_t[:], ACC[:, c, tsl(tt)]) for c in range(C)], ["consts"] + [("ACC", c) for c in range(C)], bm)
                for c in range(C):
                    ln_sq(ACC[:, c, tsl(tt)], [("ACC", c)], c, C, ONESF_t[:], [MS[4], MS[5]], bq, "msq")
                return bm, bq

            LT = [(scr(1536, TW), ["M2", "GS"]), (scr(2048, TW), ["GM", "GK"]), (scr(2560, TW), ["TH", "CMB"])]

            def m0(j):
                tt, c = divmod(j, C)
                if c == 0:
                    bm, bq = (4, 5) if tt % 2 == 0 else (6, 7)
                    if tt + 1 < NTT:
                        ln2_stats(tt + 1)
                    ln_finish(MS[0], MS[1], MS[2], MS[3], MSn, bm, bq)
                a, an = LT[j % 3]
                tt_op("dve", a, ACC[:, c, tsl(tt)], MS[1], ALU.mult, [("ACC", c), "ms1"], an)
                tt_op("dve", a, a, MS[2], ALU.subtract, an + ["ms2"], an)

            def m1(j, l=l):
                tt, c = divmod(j, C)
                a, an = LT[j % 3]
                if last:
                    act(ACC[:, c, tsl(tt)], a, AF.Identity, an + ["par"], [("ACCo", c, tt)],
                        bias=pcol(l, P_L2B + c), scale=pcol(l, P_L2G + c))
                else:
                    act(a, a, AF.Identity, an + ["par"], an, bias=pcol(l, P_L2B + c), scale=pcol(l, P_L2G + c))
                    S.op("act", lambda e: e.copy(out=XH[:, c, tsl(tt)], in_=a), reads=an, writes=[("XH", c, tt)])

            def m2(j):
                tt, c = divmod(j, C)
                a, an = LT[j % 3]
                if last:
                    S.dma("sp", "st", lambda e: e.dma_start(out=y_d[:, c * T + tt * TW:c * T + (tt + 1) * TW],
                                                            in_=ACC[:, c, tsl(tt)]), reads=[("ACCo", c, tt)])
                else:
                    tt_op("pool", XL[:, c, tsl(tt)], a, XH[:, c, tsl(tt)], ALU.subtract, an + [("XH", c, tt)], [("XL", c, tt)])

            HILO_ENG[0] = "pool"
            ln2_stats(0)
            mst = [m0, m1, m2]
            for step in range(NI + len(mst)):
                for k in reversed(range(len(mst))):
                    j = step - k
                    if 0 <= j < NI:
                        mst[k](j)
            nrot[0] = 6
            HILO_ENG[0] = "dve"
        S.final_wait("sp", ["st"])
        S.emit(E)
    return nc


def _pack_params(inp):
    P = np.zeros((128, DEPTH * NPL), np.float32)
    f = lambda a: np.ascontiguousarray(np.asarray(a, np.float32))
    for l in range(DEPTH):
        o = l * NPL
        P[:, o + P_BIN:o + P_BIN + 72] = f(inp["b_in"])[l].reshape(72, 128).T
        caw = f(inp["conv_a_w"])[l].reshape(31, 8, 128)
        P[:, o + P_CAW:o + P_CAW + 248] = caw.transpose(2, 0, 1).reshape(128, 248)
        P[:, o + P_CAB:o + P_CAB + 8] = f(inp["conv_a_b"])[l].reshape(8, 128).T
        P[:, o + P_LAG:o + P_LAG + 8] = f(inp["ln_a_g"])[l].reshape(8, 128).T
        P[:, o + P_LAB:o + P_LAB + 8] = f(inp["ln_a_b"])[l].reshape(8, 128).T
        cbw = f(inp["conv_b_w"])[l].reshape(3, 8, 128)
        P[:, o + P_CBW:o + P_CBW + 24] = cbw.transpose(2, 0, 1).reshape(128, 24)
        P[:, o + P_CS:o + P_CS + 8] = f(inp["c_scale"])[l].reshape(8, 128).T
        P[:, o + P_BO:o + P_BO + 8] = f(inp["b_o"])[l].reshape(8, 128).T
        P[:, o + P_L1G:o + P_L1G + 8] = f(inp["ln1_g"])[l].reshape(8, 128).T
        P[:, o + P_L1B:o + P_L1B + 8] = f(inp["ln1_b"])[l].reshape(8, 128).T
        P[:, o + P_L2G:o + P_L2G + 8] = f(inp["ln2_g"])[l].reshape(8, 128).T
        P[:, o + P_L2B:o + P_L2B + 8] = f(inp["ln2_b"])[l].reshape(8, 128).T
    return P


_NC_CACHE = {}


def make_in_maps(inputs, cores):
    f = lambda a: np.ascontiguousarray(np.asarray(a, np.float32))
    x = f(inputs["x"])
    shared = {
        "w_in": f(inputs["w_in"]), "w_a_out": f(inputs["w_a_out"]), "w_b_out": f(inputs["w_b_out"]),
        "w_c_group": f(inputs["w_c_group"]), "w_o": f(inputs["w_o"]),
        "w_exp_gate": f(inputs["w_exp_gate"]), "w_exp_up": f(inputs["w_exp_up"]), "w_exp_down": f(inputs["w_exp_down"]),
        "params": _pack_params(inputs),
        "wr": np.ascontiguousarray(f(inputs["w_router"]).reshape(8, 128, NE).transpose(1, 0, 2).reshape(128, 8 * NE)),
        "br": np.ascontiguousarray(np.broadcast_to(f(inputs["b_router"])[None, :], (128, NE))),
    }
    maps = []
    for b in cores:
        m = dict(shared)
        m["xT"] = np.ascontiguousarray(x[b].T)
        maps.append(m)
    return maps


def kernel(**inputs):
    n = 8
    if "nc" not in _NC_CACHE:
        _NC_CACHE["nc"] = build_program()
    nc = _NC_CACHE["nc"]
    in_maps = make_in_maps(inputs, list(range(n)))
    res = run_bass_kernel_spmd(nc, in_maps, core_ids=list(range(n)))
    out = np.empty((n, T, D), np.float32)
    for b in range(n):
        yT = np.asarray(res.results[b]["yT"]).reshape(128, C, T)
        out[b] = yT.transpose(2, 1, 0).reshape(T, D)
    return out
```
